# Optimizing a Trainium2 kernel written in Bass

```python
import math
import jax
import jax.numpy as jnp
from jax import lax
import numpy as np

D_MODEL = 1024
BATCH = 4
SEQ = 8192
DEPTH = 1

N_MEM = 256
Q_BLOCK = 128
RMS_EPS = 1e-6
NEG_INF = -1e30
ROPE_THETA = 10000.0
POS_OFFSET_MAX = 1024

DIFF_HEADS = 8
DIFF_HALF_DIM = 32
DIFF_V_DIM = 2 * DIFF_HALF_DIM
DIFF_QK_WIDTH = DIFF_HEADS * 2 * DIFF_HALF_DIM
DIFF_WIDTH = DIFF_HEADS * DIFF_V_DIM

MLA_HEADS = 8
MLA_Q_RANK = 256
MLA_KV_RANK = 128
MLA_NOPE_DIM = 64
MLA_ROPE_DIM = 32
MLA_QK_DIM = MLA_NOPE_DIM + MLA_ROPE_DIM
MLA_V_DIM = 64
MLA_WIDTH = MLA_HEADS * MLA_V_DIM

MIX_WIDTH = DIFF_WIDTH + MLA_WIDTH
IN_SPLITS = (DIFF_QK_WIDTH,
             2 * DIFF_QK_WIDTH,
             2 * DIFF_QK_WIDTH + DIFF_WIDTH,
             2 * DIFF_QK_WIDTH + DIFF_WIDTH + MLA_Q_RANK,
             2 * DIFF_QK_WIDTH + DIFF_WIDTH + MLA_Q_RANK + MLA_KV_RANK)
IN_PROJ_WIDTH = IN_SPLITS[-1] + MLA_ROPE_DIM

MEM_HEADS = 4
MEM_HEAD_DIM = 128
MEM_WIDTH = MEM_HEADS * MEM_HEAD_DIM

N_GROUPS = 4
EXPERTS_PER_GROUP = 8
N_EXPERTS = N_GROUPS * EXPERTS_PER_GROUP
TOP_K_IN_GROUP = 2
EXPERT_FF = 256
EXPERT_BLOCK = 128

kernel_name = 'hybrid_diffattn_mla_hier_moe'


def _rms_norm(x, gain):
    xf = x.astype(jnp.float32)
    y = xf * lax.rsqrt(jnp.mean(xf * xf, axis=-1, keepdims=True) + RMS_EPS)
    return (y * gain.astype(jnp.float32)).astype(x.dtype)


def _alibi_slopes(n_heads):
    return 2.0 ** (-8.0 * jnp.arange(1, n_heads + 1, dtype=jnp.float32) / n_heads)


def _rope_angles(positions, dim):
    inv_freq = ROPE_THETA ** (-jnp.arange(0, dim, 2, dtype=jnp.float32) / dim)
    ang = positions.astype(jnp.float32)[..., None] * inv_freq
    return jnp.cos(ang), jnp.sin(ang)


def _apply_rope(x, cos, sin):
    half = x.shape[-1] // 2
    x1 = x[..., :half].astype(jnp.float32)
    x2 = x[..., half:].astype(jnp.float32)
    return jnp.concatenate([x1 * cos - x2 * sin, x2 * cos + x1 * sin], axis=-1).astype(x.dtype)


def _causal_mask(start, seq_len):
    q_idx = start + jnp.arange(Q_BLOCK)
    k_idx = jnp.arange(seq_len)
    return k_idx[None, :] <= q_idx[:, None]


def _query_block_sweep(block_fn, seq_len):
    n_blocks = seq_len // Q_BLOCK
    out = lax.map(block_fn, jnp.arange(n_blocks))
    nb, b, qb, w = out.shape
    return out.transpose(1, 0, 2, 3).reshape(b, nb * qb, w)


def _differential_attention(q, k, v, positions, lam, lambda_init, out_gain):
    b, s, h, _ = q.shape
    q = q.transpose(0, 2, 1, 3)
    k = k.transpose(0, 2, 1, 3)
    v = v.transpose(0, 2, 1, 3)
    q1, q2 = q[..., :DIFF_HALF_DIM], q[..., DIFF_HALF_DIM:]
    k1, k2 = k[..., :DIFF_HALF_DIM], k[..., DIFF_HALF_DIM:]
    slopes = _alibi_slopes(h)
    scale = DIFF_HALF_DIM ** -0.5
    pos_f = positions.astype(jnp.float32)
    gain = out_gain.astype(jnp.float32) * (1.0 - lambda_init)

    def block_fn(i):
        start = i * Q_BLOCK
        qb1 = lax.dynamic_slice_in_dim(q1, start, Q_BLOCK, axis=2)
        qb2 = lax.dynamic_slice_in_dim(q2, start, Q_BLOCK, axis=2)
        pos_q = lax.dynamic_slice_in_dim(pos_f, start, Q_BLOCK, axis=1)
        dist = jnp.abs(pos_q[:, :, None] - pos_f[:, None, :])
        bias = -slopes[None, :, None, None] * dist[:, None]
        mask = _causal_mask(start, s)

        def attn_map(qb, kk):
            sc = jnp.einsum('bhqd,bhkd->bhqk', qb, kk, preferred_element_type=jnp.float32) * scale + bias
            return jax.nn.softmax(jnp.where(mask, sc, NEG_INF), axis=-1)

        a = attn_map(qb1, k1) - lam * attn_map(qb2, k2)
        o = jnp.einsum('bhqk,bhkd->bhqd', a.astype(v.dtype), v, preferred_element_type=jnp.float32)
        o = o * lax.rsqrt(jnp.mean(o * o, axis=-1, keepdims=True) + RMS_EPS) * gain
        return o.transpose(0, 2, 1, 3).reshape(b, Q_BLOCK, h * DIFF_V_DIM).astype(v.dtype)

    return _query_block_sweep(block_fn, s)


def _latent_attention(q, k, v):
    b, s, h, _ = q.shape
    q = q.transpose(0, 2, 1, 3)
    k = k.transpose(0, 2, 1, 3)
    v = v.transpose(0, 2, 1, 3)
    scale = MLA_QK_DIM ** -0.5

    def block_fn(i):
        start = i * Q_BLOCK
        qb = lax.dynamic_slice_in_dim(q, start, Q_BLOCK, axis=2)
        sc = jnp.einsum('bhqd,bhkd->bhqk', qb, k, preferred_element_type=jnp.float32) * scale
        p = jax.nn.softmax(jnp.where(_causal_mask(start, s), sc, NEG_INF), axis=-1)
        o = jnp.einsum('bhqk,bhkd->bhqd', p.astype(v.dtype), v, preferred_element_type=jnp.float32)
        return o.transpose(0, 2, 1, 3).reshape(b, Q_BLOCK, h * MLA_V_DIM).astype(v.dtype)

    return _query_block_sweep(block_fn, s)


def _memory_cross_attention(h, mem_n, w_q, w_kv, w_o):
    b, s, _ = h.shape
    n_mem = mem_n.shape[1]
    q = (h @ w_q).reshape(b, s, MEM_HEADS, MEM_HEAD_DIM)
    kv = (mem_n @ w_kv).reshape(b, n_mem, 2, MEM_HEADS, MEM_HEAD_DIM)
    k, v = kv[:, :, 0], kv[:, :, 1]
    sc = jnp.einsum('bshd,bmhd->bhsm', q, k, preferred_element_type=jnp.float32) * MEM_HEAD_DIM ** -0.5
    p = jax.nn.softmax(sc, axis=-1)
    o = jnp.einsum('bhsm,bmhd->bshd', p.astype(v.dtype), v).reshape(b, s, MEM_WIDTH)
    return o @ w_o


def _hierarchical_moe(h, w_group_router, b_group_router, w_expert_router, b_expert_router,
                      w_gate, w_up, w_down):
    b, s, d = h.shape
    n_tok = b * s
    hf = h.reshape(n_tok, d)
    g_logits = jnp.einsum('td,dg->tg', hf, w_group_router, preferred_element_type=jnp.float32) \
        + b_group_router.astype(jnp.float32)
    g_probs = jax.nn.softmax(g_logits, axis=-1)
    g_gate, g_sel = lax.top_k(g_probs, 1)
    e_logits = jnp.einsum('td,de->te', hf, w_expert_router, preferred_element_type=jnp.float32) \
        + b_expert_router.astype(jnp.float32)
    e_logits = e_logits.reshape(n_tok, N_GROUPS, EXPERTS_PER_GROUP)
    e_logits = jnp.take_along_axis(e_logits, g_sel[:, :, None], axis=1)[:, 0]
    e_probs = jax.nn.softmax(e_logits, axis=-1)
    e_gate, e_sel = lax.top_k(e_probs, TOP_K_IN_GROUP)
    e_gate = e_gate / jnp.sum(e_gate, axis=-1, keepdims=True)
    weights = (g_gate * e_gate).reshape(-1)
    expert_ids = (g_sel * EXPERTS_PER_GROUP + e_sel).reshape(-1)
    token_ids = jnp.repeat(jnp.arange(n_tok, dtype=jnp.int32), TOP_K_IN_GROUP)

    n_assign = n_tok * TOP_K_IN_GROUP
    n_blocks = -(-(n_assign + N_EXPERTS * (EXPERT_BLOCK - 1)) // EXPERT_BLOCK)
    n_slots = n_blocks * EXPERT_BLOCK
    order = jnp.argsort(expert_ids)
    sorted_e = expert_ids[order]
    counts = jnp.bincount(expert_ids, length=N_EXPERTS)
    padded = ((counts + EXPERT_BLOCK - 1) // EXPERT_BLOCK) * EXPERT_BLOCK
    seg_start = jnp.cumsum(counts) - counts
    padded_end = jnp.cumsum(padded)
    padded_start = padded_end - padded
    dest = padded_start[sorted_e] + (jnp.arange(n_assign) - seg_start[sorted_e])
    slot_token = jnp.full((n_slots,), n_tok, jnp.int32).at[dest].set(token_ids[order])
    slot_weight = jnp.zeros((n_slots,), jnp.float32).at[dest].set(weights[order])
    block_expert = jnp.minimum(
        jnp.searchsorted(padded_end, jnp.arange(n_blocks) * EXPERT_BLOCK, side='right'), N_EXPERTS - 1)
    h_pad = jnp.concatenate([hf, jnp.zeros((1, d), hf.dtype)], axis=0)

    def block_fn(args):
        tok, wt, e = args
        xb = h_pad[tok]
        hidden = jax.nn.silu(xb @ w_gate[e]) * (xb @ w_up[e])
        return (hidden @ w_down[e]) * wt[:, None].astype(hf.dtype)

    y = lax.map(block_fn, (slot_token.reshape(n_blocks, EXPERT_BLOCK),
                           slot_weight.reshape(n_blocks, EXPERT_BLOCK),
                           block_expert))
    out = jnp.zeros((n_tok + 1, d), hf.dtype).at[slot_token].add(y.reshape(n_slots, d))[:n_tok]
    return out.reshape(b, s, d)


def setup_inputs(seed: int = 0) -> dict:
    key = jax.random.key(seed)
    ks = jax.random.split(key, 30)

    def w(k, shape, fan_in):
        return jax.random.normal(k, shape, jnp.float32) * fan_in ** -0.5

    def gain(k, shape):
        return 1.0 + 0.02 * jax.random.normal(k, shape, jnp.float32)

    x = jax.random.normal(ks[0], (BATCH, SEQ, D_MODEL), jnp.float32)
    mem = jax.random.normal(ks[1], (BATCH, N_MEM, D_MODEL), jnp.float32)
    offset = jax.random.randint(ks[2], (BATCH, 1), 0, POS_OFFSET_MAX, jnp.int32)
    positions = (offset + jnp.arange(SEQ, dtype=jnp.int32)[None, :]).astype(jnp.int32)
    return {
        'x': x,
        'mem': mem,
        'positions': positions,
        'norm_mix_g': gain(ks[3], (DEPTH, D_MODEL)),
        'w_in': w(ks[4], (DEPTH, D_MODEL, IN_PROJ_WIDTH), D_MODEL),
        'diff_lambda_q1': 0.1 * jax.random.normal(ks[5], (DEPTH, DIFF_HALF_DIM), jnp.float32),
        'diff_lambda_k1': 0.1 * jax.random.normal(ks[6], (DEPTH, DIFF_HALF_DIM), jnp.float32),
        'diff_lambda_q2': 0.1 * jax.random.normal(ks[7], (DEPTH, DIFF_HALF_DIM), jnp.float32),
        'diff_lambda_k2': 0.1 * jax.random.normal(ks[8], (DEPTH, DIFF_HALF_DIM), jnp.float32),
        'diff_out_g': gain(ks[9], (DEPTH, DIFF_V_DIM)),
        'mla_q_norm_g': gain(ks[10], (DEPTH, MLA_Q_RANK)),
        'w_mla_uq': w(ks[11], (DEPTH, MLA_Q_RANK, MLA_HEADS * MLA_QK_DIM), MLA_Q_RANK),
        'mla_kv_norm_g': gain(ks[12], (DEPTH, MLA_KV_RANK)),
        'w_mla_ukv': w(ks[13], (DEPTH, MLA_KV_RANK, MLA_HEADS * (MLA_NOPE_DIM + MLA_V_DIM)), MLA_KV_RANK),
        'mla_out_g': gain(ks[14], (DEPTH, MLA_WIDTH)),
        'w_out': w(ks[15], (DEPTH, MIX_WIDTH, D_MODEL), MIX_WIDTH),
        'norm_cross_g': gain(ks[16], (DEPTH, D_MODEL)),
        'norm_mem_g': gain(ks[17], (DEPTH, D_MODEL)),
        'w_mem_q': w(ks[18], (DEPTH, D_MODEL, MEM_WIDTH), D_MODEL),
        'w_mem_kv': w(ks[19], (DEPTH, D_MODEL, 2 * MEM_WIDTH), D_MODEL),
        'w_mem_o': w(ks[20], (DEPTH, MEM_WIDTH, D_MODEL), MEM_WIDTH),
        'norm_ffn_g': gain(ks[21], (DEPTH, D_MODEL)),
        'w_group_router': w(ks[22], (DEPTH, D_MODEL, N_GROUPS), D_MODEL),
        'b_group_router': 0.01 * jax.random.normal(ks[23], (DEPTH, N_GROUPS), jnp.float32),
        'w_expert_router': w(ks[24], (DEPTH, D_MODEL, N_EXPERTS), D_MODEL),
        'b_expert_router': 0.01 * jax.random.normal(ks[25], (DEPTH, N_EXPERTS), jnp.float32),
        'w_expert_gate': w(ks[26], (DEPTH, N_EXPERTS, D_MODEL, EXPERT_FF), D_MODEL),
        'w_expert_up': w(ks[27], (DEPTH, N_EXPERTS, D_MODEL, EXPERT_FF), D_MODEL),
        'w_expert_down': w(ks[28], (DEPTH, N_EXPERTS, EXPERT_FF, D_MODEL), EXPERT_FF),
        'norm_final_g': gain(ks[29], (D_MODEL,)),
    }


def reference(x, mem, positions, norm_mix_g, w_in, diff_lambda_q1, diff_lambda_k1,
              diff_lambda_q2, diff_lambda_k2, diff_out_g, mla_q_norm_g, w_mla_uq,
              mla_kv_norm_g, w_mla_ukv, mla_out_g, w_out, norm_cross_g, norm_mem_g,
              w_mem_q, w_mem_kv, w_mem_o, norm_ffn_g, w_group_router, b_group_router,
              w_expert_router, b_expert_router, w_expert_gate, w_expert_up, w_expert_down,
              norm_final_g):
    b, s, _ = x.shape
    cos, sin = _rope_angles(positions, MLA_ROPE_DIM)
    for l in range(DEPTH):
        h = _rms_norm(x, norm_mix_g[l])
        proj = h @ w_in[l]
        q_d, k_d, v_d, c_q, c_kv, k_pe = jnp.split(proj, IN_SPLITS, axis=-1)

        lambda_init = 0.8 - 0.6 * math.exp(-0.3 * l)
        lam = (jnp.exp(jnp.sum(diff_lambda_q1[l].astype(jnp.float32) * diff_lambda_k1[l].astype(jnp.float32)))
               - jnp.exp(jnp.sum(diff_lambda_q2[l].astype(jnp.float32) * diff_lambda_k2[l].astype(jnp.float32)))
               + lambda_init)
        diff_out = _differential_attention(
            q_d.reshape(b, s, DIFF_HEADS, 2 * DIFF_HALF_DIM),
            k_d.reshape(b, s, DIFF_HEADS, 2 * DIFF_HALF_DIM),
            v_d.reshape(b, s, DIFF_HEADS, DIFF_V_DIM),
            positions, lam, lambda_init, diff_out_g[l])

        q_m = (_rms_norm(c_q, mla_q_norm_g[l]) @ w_mla_uq[l]).reshape(b, s, MLA_HEADS, MLA_QK_DIM)
        q_nope, q_pe = q_m[..., :MLA_NOPE_DIM], q_m[..., MLA_NOPE_DIM:]
        q_pe = _apply_rope(q_pe, cos[:, :, None, :], sin[:, :, None, :])
        q_m = jnp.concatenate([q_nope, q_pe], axis=-1)
        kv_m = (_rms_norm(c_kv, mla_kv_norm_g[l]) @ w_mla_ukv[l]).reshape(
            b, s, MLA_HEADS, MLA_NOPE_DIM + MLA_V_DIM)
        k_nope, v_m = kv_m[..., :MLA_NOPE_DIM], kv_m[..., MLA_NOPE_DIM:]
        k_pe = _apply_rope(k_pe, cos, sin)
        k_m = jnp.concatenate(
            [k_nope, jnp.broadcast_to(k_pe[:, :, None, :], (b, s, MLA_HEADS, MLA_ROPE_DIM))], axis=-1)
        mla_out = _rms_norm(_latent_attention(q_m, k_m, v_m), mla_out_g[l])

        x = x + jnp.concatenate([diff_out, mla_out], axis=-1) @ w_out[l]

        x = x + _memory_cross_attention(_rms_norm(x, norm_cross_g[l]), _rms_norm(mem, norm_mem_g[l]),
                                        w_mem_q[l], w_mem_kv[l], w_mem_o[l])

        x = x + _hierarchical_moe(_rms_norm(x, norm_ffn_g[l]), w_group_router[l], b_group_router[l],
                                  w_expert_router[l], b_expert_router[l],
                                  w_expert_gate[l], w_expert_up[l], w_expert_down[l])
    return _rms_norm(x, norm_final_g)
```

```python
import contextlib
import os
import numpy as np
import ml_dtypes
import concourse.bass as bass
import concourse.mybir as mybir
from concourse.bass_utils import run_bass_kernel_spmd

F32 = mybir.dt.float32
BF16 = mybir.dt.bfloat16
I32 = mybir.dt.int32
AF = mybir.ActivationFunctionType
ALU = mybir.AluOpType

NEG = -30000.0
EPS = 1e-6
S_ALL = 8192
S_OWN = 4096
NT_ALL = 64
NT_OWN = 32
PI = float(np.pi)
TWO_PI = float(2 * np.pi)
SLOPES = [2.0 ** (-(h + 1)) for h in range(8)]
INV_FREQ = [float(np.float32(10000.0) ** np.float32(-(2 * j) / 32.0)) for j in range(16)]
SC_D = float(32 ** -0.5)
SC_M = float(96 ** -0.5)
SC_X = float(128 ** -0.5)

ENGS = ("pe", "act", "dve", "pool", "sp")


class Sched:
    csem = None
    cbase = None
    pools = None

    @classmethod
    def reset(cls, nc):
        cls.csem = {e: nc.alloc_semaphore(name="c_" + e) for e in ENGS}
        cls.cbase = {e: 0 for e in ENGS}
        cls.pools = {"sw": [], "hw": []}
        cls.nalloc = 0

    def __init__(self, nc, same_engine_sync=True):
        self.nc = nc
        self.same = same_engine_sync
        self.ops = {e: [] for e in ENGS}
        self.count = dict(Sched.cbase)
        self.last_w = {}
        self.readers = {}
        self.dma_keys = {}

    def _deps(self, reads, writes):
        deps = []
        for k in reads:
            t = self.last_w.get(k)
            if t is not None:
                deps.append(t)
        for k in writes:
            t = self.last_w.get(k)
            if t is not None:
                deps.append(t)
            deps.extend(self.readers.get(k, ()))
        return deps

    def _commit(self, tok, reads, writes):
        for k in reads:
            self.readers.setdefault(k, []).append(tok)
        for k in writes:
            self.last_w[k] = tok
            self.readers[k] = []

    def op(self, eng, fn, reads=(), writes=()):
        deps = self._deps(reads, writes)
        self.count[eng] += 1
        tok = ("c", eng, self.count[eng])
        self.ops[eng].append((fn, deps, tok))
        self._commit(tok, reads, writes)
        return tok

    def dma(self, eng, fn, reads=(), writes=(), semkey=None):
        deps = self._deps(reads, writes)
        if semkey is None:
            semkey = tuple(writes)
        if semkey not in self.dma_keys:
            pname = "sw" if eng == "pool" else "hw"
            pool = Sched.pools[pname]
            if pool:
                pool.sort(key=lambda x: -x[1])
                handle, cnt = pool.pop()
            else:
                Sched.nalloc += 1
                handle, cnt = self.nc.alloc_semaphore(name="d_%d" % Sched.nalloc), 0
            self.dma_keys[semkey] = [handle, cnt, eng, pname]
        ent = self.dma_keys[semkey]
        assert ent[2] == eng, "dma key used from two queues: %s" % (semkey,)
        ent[1] += 1
        tok = ("d", semkey, ent[1])
        self.ops[eng].append((fn, deps, tok))
        self._commit(tok, reads, writes)
        return tok

    def emit(self):
        nc = self.nc
        csem = Sched.csem
        dk = self.dma_keys
        finals = [(ent[0], ent[1]) for ent in dk.values()]
        cfinal = dict(self.count)
        with nc.Block() as block:

            def body(eng_name):
                def run(eng):
                    seen = {}
                    for fn, deps, tok in self.ops[eng_name]:
                        need = {}
                        for d in deps:
                            if d[0] == "c":
                                if d[1] == eng_name and (not self.same or eng_name == "pe"):
                                    continue
                                key = ("c", d[1])
                                val = d[2]
                            else:
                                key = ("d", d[1])
                                val = 16 * d[2]
                            if val > need.get(key, 0):
                                need[key] = val
                        for key, val in need.items():
                            if seen.get(key, 0) >= val:
                                continue
                            seen[key] = val
                            sem = csem[key[1]] if key[0] == "c" else dk[key[1]][0]
                            eng.wait_ge(sem, val)
                        ins = fn(eng)
                        if tok[0] == "c":
                            ins.then_inc(csem[eng_name], 1)
                        else:
                            ins.then_inc(dk[tok[1]][0], 16)
                    for handle, cnt in finals:
                        eng.wait_ge(handle, 16 * cnt)
                    for e2, cnt in cfinal.items():
                        if cnt > 0:
                            eng.wait_ge(csem[e2], cnt)
                return run

            block.tensor(body("pe"))
            block.scalar(body("act"))
            block.vector(body("dve"))
            block.gpsimd(body("pool"))
            block.sync(body("sp"))
        Sched.cbase = dict(self.count)
        for ent in dk.values():
            Sched.pools[ent[3]].append([ent[0], ent[1]])


class Ctx:
    pass


def declare_io(nc, debug, upto=99):
    D = Ctx()
    D.in_names = []

    def inp(name, shape, dt=F32):
        if upto < 5 and name in ("w_gate", "w_up", "w_down"):
            return None
        D.in_names.append(name)
        return nc.dram_tensor(name, list(shape), dt, kind="ExternalInput").ap()
    D.xk = inp("xk", [S_ALL, 1024])
    D.postok = inp("postok", [128, NT_ALL], I32)
    D.mem = inp("mem", [256, 1024])
    D.flagmask = inp("flagmask", [128, 128])
    D.ident = inp("ident", [128, 128])
    D.trimask = inp("trimask", [128, 128])
    D.norm_mix_g = inp("norm_mix_g", [1, 1024])
    D.w_in = inp("w_in", [1024, 1952])
    D.lam_q1 = inp("lam_q1", [1, 32]); D.lam_k1 = inp("lam_k1", [1, 32])
    D.lam_q2 = inp("lam_q2", [1, 32]); D.lam_k2 = inp("lam_k2", [1, 32])
    D.diff_out_g = inp("diff_out_g", [1, 64])
    D.mla_q_norm_g = inp("mla_q_norm_g", [1, 256])
    D.w_mla_uq = inp("w_mla_uq", [256, 768])
    D.mla_kv_norm_g = inp("mla_kv_norm_g", [1, 128])
    D.w_mla_ukv = inp("w_mla_ukv", [128, 1024])
    D.mla_out_g = inp("mla_out_g", [1, 512])
    D.w_out = inp("w_out", [1024, 1024])
    D.norm_cross_g = inp("norm_cross_g", [1, 1024])
    D.norm_mem_g = inp("norm_mem_g", [1, 1024])
    D.w_mem_q = inp("w_mem_q", [1024, 512])
    D.w_mem_kv = inp("w_mem_kv", [1024, 1024])
    D.w_mem_o = inp("w_mem_o", [512, 1024])
    D.norm_ffn_g = inp("norm_ffn_g", [1, 1024])
    D.w_grp = inp("w_grp", [1024, 4]); D.b_grp = inp("b_grp", [1, 4])
    D.w_exr = inp("w_exr", [1024, 32]); D.b_exr = inp("b_exr", [1, 32])
    D.w_gate = inp("w_gate", [32, 1024, 256])
    D.w_up = inp("w_up", [32, 1024, 256])
    D.w_down = inp("w_down", [32, 256, 1024])
    D.norm_final_g = inp("norm_final_g", [1, 1024])
    D.out = nc.dram_tensor("out", [S_OWN, 1024], F32, kind="ExternalOutput").ap()
    sk = "ExternalOutput" if debug else "Internal"
    scr = lambda name, shape, dt: nc.dram_tensor(name, list(shape), dt, kind=sk).ap()
    D.KdTc = scr("KdTc", [4, 128, S_ALL], BF16)
    D.QdTc = scr("QdTc", [4, 128, S_OWN], BF16)
    D.Vd = scr("Vd", [S_ALL, 520], BF16)
    D.KmT = scr("KmT", [8, 96, S_ALL], BF16)
    D.QmT = scr("QmT", [8, 96, S_OWN], BF16)
    D.Vm = scr("Vm", [S_ALL, 520], BF16)
    D.krow = scr("krow", [4, S_ALL], BF16)
    D.qrow = scr("qrow", [8, 4, S_OWN], BF16)
    D.AO = scr("AO", [S_OWN, 1024], BF16)
    D.SSM = scr("SSM", [128, NT_OWN], F32)
    D.X2 = scr("X2", [S_OWN, 1024], F32)
    D.H3T = scr("H3T", [128, 8, S_OWN], BF16)
    D.WRD = scr("WRD", [128, NT_OWN, 32], F32)
    return D


def rstd_ops(S, ss, rstd, n, key_ss, key_r):
    S.op("act", lambda e: e.activation(out=rstd, in_=ss, func=AF.Ln, scale=1.0 / n, bias=EPS),
         reads=[key_ss], writes=[key_r])
    S.op("act", lambda e: e.activation(out=rstd, in_=rstd, func=AF.Exp, scale=-0.5),
         reads=[key_r], writes=[key_r])


def phase1(nc, D):
    with contextlib.ExitStack() as st:
        T = lambda name, shape, dt: st.enter_context(nc.sbuf_tensor(name, list(shape), dt))
        PS = lambda name, shape, dt: st.enter_context(nc.psum_tensor(name, list(shape), dt))
        S = Sched(nc)
        idf = T("idf", [128, 128], F32)
        idb = T("idb", [128, 128], BF16)
        g_mix = T("g_mix", [128, 1024], F32)
        g_q = T("g_q", [128, 256], F32)
        g_kv = T("g_kv", [128, 128], F32)
        wb_in = T("wb_in", [128, 8, 1952], BF16)
        wb_uq = T("wb_uq", [128, 2, 768], BF16)
        wb_ukv = T("wb_ukv", [128, 1024], BF16)
        posi = T("posi", [128, NT_ALL], I32)
        posf = T("posf", [128, NT_ALL], F32)
        pa = T("pa", [128, NT_ALL], F32)
        pb = T("pb", [128, NT_ALL], F32)
        ang = T("ang", [128, NT_ALL, 16], F32)
        angm = T("angm", [128, NT_ALL, 16], F32)
        angi = T("angi", [128, NT_ALL, 16], I32)
        angf = T("angf", [128, NT_ALL, 16], F32)
        cosk = T("cosk", [128, NT_ALL, 16], F32)
        sink = T("sink", [128, NT_ALL, 16], F32)
        cosq = T("cosq", [128, NT_OWN, 16], F32)
        sinq = T("sinq", [128, NT_OWN, 16], F32)
        rowt = T("rowt", [128, 128], F32)
        cosq8 = T("cosq8", [128, NT_OWN, 8, 16], F32)
        sinq8 = T("sinq8", [128, NT_OWN, 8, 16], F32)
        qpe = T("qpe", [128, 8, 32], F32)
        rowb = T("rowb", [128, 128], BF16)
        xt = [T("xt%d" % i, [128, 1024], F32) for i in range(2)]
        junk = T("junk", [128, 1024], BF16)
        ssl = [T("ss%d" % i, [128, 1], F32) for i in range(3)]
        rsl = [T("rs%d" % i, [128, 1], F32) for i in range(3)]
        hn = T("hn", [128, 1024], BF16)
        hT = [T("hT%d" % i, [128, 8, 512], BF16) for i in range(2)]
        vd_t = [T("vd_t%d" % i, [128, 8, 65], BF16) for i in range(2)]
        vm_t = [T("vm_t%d" % i, [128, 8, 65], BF16) for i in range(2)]
        km_t = T("km_t", [128, 8, 96], BF16)
        qm_t = T("qm_t", [128, 8, 96], BF16)
        ckvn = T("ckvn", [128, 128], BF16)
        cqn = T("cqn", [128, 256], BF16)
        ckvnT = T("ckvnT", [128, 128], BF16)
        cqnT = T("cqnT", [128, 2, 128], BF16)
        kpe = T("kpe", [128, 32], F32)
        kr = T("kr", [128, 32], BF16)
        tmpa = T("tmpa", [128, 8, 16], F32)
        tmpb = T("tmpb", [128, 8, 16], F32)
        kmT = [T("kmT%d" % i, [96, 8, 512], BF16) for i in range(2)]
        qmT = [T("qmT%d" % i, [96, 8, 512], BF16) for i in range(2)]
        kdT = [T("kdT%d" % i, [128, 512], BF16) for i in range(4)]

        p_tr = PS("p_tr", [128, 8, 128], BF16)
        p_vd = PS("p_vd", [128, 512], F32)
        p_lat = PS("p_lat", [128, 512], F32)
        p_st = PS("p_st", [128, 8, 128], BF16)
        p_kv = PS("p_kv", [128, 1024], F32)
        p_mt = PS("p_mt", [128, 8, 128], BF16)
        p_fm = PS("p_fm", [128, 512], F32)

        S.dma("sp", lambda e: e.dma_start(out=idf[:], in_=D.ident[:, :]), writes=["idf"])
        S.op("dve", lambda e: e.tensor_copy(out=idb[:], in_=idf[:]), reads=["idf"], writes=["idb"])
        S.dma("sp", lambda e: e.dma_start(out=g_mix[:], in_=D.norm_mix_g.partition_broadcast(128)), writes=["g_mix"])
        S.dma("sp", lambda e: e.dma_start(out=g_q[:], in_=D.mla_q_norm_g.partition_broadcast(128)), writes=["g_q"])
        S.dma("sp", lambda e: e.dma_start(out=g_kv[:], in_=D.mla_kv_norm_g.partition_broadcast(128)), writes=["g_kv"])
        S.dma("sp", lambda e: e.dma_start(out=posi[:], in_=D.postok[:, :]), writes=["posi"])
        w_in_v = D.w_in.rearrange("(c p) n -> p c n", p=128)
        for c in range(8):
            S.dma("pool", lambda e, c=c: e.dma_start(out=wb_in[:, c, :], in_=w_in_v[:, c, :]), writes=["wb_in"])
        S.dma("pool", lambda e: e.dma_start(out=wb_uq[:], in_=D.w_mla_uq.rearrange("(c p) n -> p c n", p=128)),
              writes=["wb_uq"])
        S.dma("pool", lambda e: e.dma_start(out=wb_ukv[:], in_=D.w_mla_ukv[:, :]), writes=["wb_ukv"])

        STOP = int(os.environ.get("P1STOP", "99"))
        if STOP == 0:
            S.emit(); return
        S.op("dve", lambda e: e.tensor_copy(out=posf[:], in_=posi[:]), reads=["posi"], writes=["posf"])
        S.op("dve", lambda e: e.tensor_single_scalar(out=pa[:], in_=posf[:], scalar=1.0 / 64, op=ALU.mult),
             reads=["posf"], writes=["pa"])
        S.op("dve", lambda e: e.tensor_copy(out=posi[:], in_=pa[:]), reads=["pa", "posf"], writes=["posi"])
        S.op("dve", lambda e: e.tensor_copy(out=pb[:], in_=posi[:]), reads=["posi"], writes=["pb"])
        S.op("dve", lambda e: e.tensor_tensor(out=pa[:], in0=pa[:], in1=pb[:], op=ALU.subtract),
             reads=["pa", "pb"], writes=["pa"])
        S.op("dve", lambda e: e.tensor_single_scalar(out=pa[:], in_=pa[:], scalar=0.0, op=ALU.is_lt),
             reads=["pa"], writes=["pa"])
        S.op("dve", lambda e: e.tensor_tensor(out=pa[:], in0=pb[:], in1=pa[:], op=ALU.subtract),
             reads=["pa", "pb"], writes=["pa"])
        S.op("dve", lambda e: e.scalar_tensor_tensor(out=pb[:], in0=pa[:], scalar=-64.0, in1=posf[:],
                                                     op0=ALU.mult, op1=ALU.add),
             reads=["pa", "posf"], writes=["pb"])
        for j in range(16):
            S.op("dve", lambda e, j=j: e.tensor_single_scalar(out=ang[:, :, j], in_=posf[:], scalar=INV_FREQ[j],
                                                              op=ALU.mult), reads=["posf"], writes=["ang"])
        S.op("dve", lambda e: e.tensor_single_scalar(out=ang[:], in_=ang[:], scalar=1.0 / TWO_PI, op=ALU.mult),
             reads=["ang"], writes=["ang"])

        def sin_turns(dst, key, shift):
            S.op("dve", lambda e: e.tensor_single_scalar(out=angm[:], in_=ang[:], scalar=shift, op=ALU.add),
                 reads=["ang"], writes=["angm"])
            S.op("dve", lambda e: e.tensor_copy(out=angi[:], in_=angm[:]), reads=["angm"], writes=["angi"])
            S.op("dve", lambda e: e.tensor_copy(out=angf[:], in_=angi[:]), reads=["angi"], writes=["angf"])
            S.op("dve", lambda e: e.tensor_tensor(out=angm[:], in0=angm[:], in1=angf[:], op=ALU.subtract),
                 reads=["angm", "angf"], writes=["angm"])
            S.op("dve", lambda e: e.tensor_single_scalar(out=angf[:], in_=angm[:], scalar=0.5, op=ALU.is_ge),
                 reads=["angm"], writes=["angf"])
            S.op("dve", lambda e: e.tensor_tensor(out=angm[:], in0=angm[:], in1=angf[:], op=ALU.subtract),
                 reads=["angm", "angf"], writes=["angm"])
            S.op("act", lambda e: e.activation(out=dst, in_=angm[:], func=AF.Sin, scale=TWO_PI),
                 reads=["angm"], writes=[key])

        sin_turns(sink[:], "sink", 0.0)
        sin_turns(cosk[:], "cosk", 0.25)
        S.op("dve", lambda e: e.tensor_single_scalar(out=cosq[:], in_=cosk[:, 0:NT_OWN, :], scalar=SC_M, op=ALU.mult),
             reads=["cosk"], writes=["cosq"])
        S.op("dve", lambda e: e.tensor_single_scalar(out=sinq[:], in_=sink[:, 0:NT_OWN, :], scalar=SC_M, op=ALU.mult),
             reads=["sink"], writes=["sinq"])

        for h in range(8):
            S.op("pool", lambda e, h=h: e.tensor_copy(out=cosq8[:, :, h, :], in_=cosq[:]), reads=["cosq"], writes=["cosq8"])
            S.op("pool", lambda e, h=h: e.tensor_copy(out=sinq8[:, :, h, :], in_=sinq[:]), reads=["sinq"], writes=["sinq8"])
        def row_flush(dst_ap, npart):
            S.op("pe", lambda e: e.transpose(out=p_fm[:, 0:128], in_=rowt[:], identity=idf[:]),
                 reads=["rowt", "idf"], writes=["p_fm"])
            S.op("dve", lambda e: e.tensor_copy(out=rowb[:], in_=p_fm[:, 0:128]), reads=["p_fm"], writes=["rowb"])
            S.dma("sp", lambda e: e.dma_start(out=dst_ap, in_=rowb[0:npart, :]), reads=["rowb"], writes=["rows_dram"])

        S.op("dve", lambda e: e.tensor_copy(out=rowt[:, 0:64], in_=pa[:]), reads=["pa"], writes=["rowt"])
        S.op("dve", lambda e: e.tensor_copy(out=rowt[:, 64:128], in_=pb[:]), reads=["pb"], writes=["rowt"])
        row_flush(D.krow[0:2, :].rearrange("r (t p) -> (r t) p", p=128), 128)
        S.op("dve", lambda e: e.memset(rowt[:], 1.0), reads=["p_fm"], writes=["rowt"])
        row_flush(D.krow[2:4, :].rearrange("r (t p) -> (r t) p", p=128), 128)
        for h in range(8):
            s = SLOPES[h]
            S.op("dve", lambda e, s=s: e.memset(rowt[:, 0:32], 64.0 * s), reads=["p_fm"], writes=["rowt"])
            S.op("dve", lambda e, s=s: e.memset(rowt[:, 32:64], s), writes=["rowt"])
            S.op("dve", lambda e, s=s: e.tensor_single_scalar(out=rowt[:, 64:96], in_=pa[:, 0:NT_OWN], scalar=-64.0 * s,
                                                              op=ALU.mult), reads=["pa"], writes=["rowt"])
            S.op("dve", lambda e, s=s: e.tensor_single_scalar(out=rowt[:, 96:128], in_=pb[:, 0:NT_OWN], scalar=-s,
                                                              op=ALU.mult), reads=["pb"], writes=["rowt"])
            row_flush(D.qrow[h].rearrange("r (t p) -> (r t) p", p=128), 128)
        if STOP == 1:
            S.emit(); return
        if STOP == 2:
            S.emit(); return
        for i in range(2):
            S.op("pool", lambda e, i=i: e.memset(vd_t[i][:], 1.0), writes=["vd_t%d" % i])
            S.op("pool", lambda e, i=i: e.memset(vm_t[i][:], 1.0), writes=["vm_t%d" % i])

        NTL = int(os.environ.get("P1NT", str(NT_ALL)))
        CUT = int(os.environ.get("P1CUT", "99"))
        flushq = []
        fcnt = [0]
        hn2 = [hn, T("hn_b", [128, 1024], BF16)]
        ssx = [T("ssx%d" % i, [128, 1], F32) for i in range(2)]
        rsx = [T("rsx%d" % i, [128, 1], F32) for i in range(2)]

        def norm_x(tt):
            xb_ = tt % 2
            S.op("act", lambda e: e.activation(out=junk[:], in_=xt[xb_][:], func=AF.Square, accum_out=ssx[xb_][:]),
                 reads=["xt%d" % xb_], writes=["junk", "ssx%d" % xb_])
            rstd_ops(S, ssx[xb_][:], rsx[xb_][:], 1024, "ssx%d" % xb_, "rsx%d" % xb_)
            S.op("dve", lambda e: e.scalar_tensor_tensor(out=hn2[xb_][:], in0=xt[xb_][:], scalar=rsx[xb_][:], in1=g_mix[:],
                                                         op0=ALU.mult, op1=ALU.mult),
                 reads=["xt%d" % xb_, "rsx%d" % xb_, "g_mix"], writes=["hn%d" % xb_])

        for t in range(NTL):
            own = t < NT_OWN
            s_i = t // 4
            j = t % 4
            hs = s_i % 2
            xb = t % 2
            tok0 = t * 128
            if t == 0:
                S.dma("sp", lambda e: e.dma_start(out=xt[0][:], in_=D.xk[0:128, :]), writes=["xt0"])
            if t + 1 < NTL:
                S.dma("sp", lambda e, t=t: e.dma_start(out=xt[(t + 1) % 2][:], in_=D.xk[(t + 1) * 128:(t + 2) * 128, :]),
                      writes=["xt%d" % ((t + 1) % 2)])
            if t == 0:
                norm_x(0)
            for c in range(8):
                S.op("pe", lambda e, c=c, xb=xb: e.transpose(out=p_tr[:, c, :], in_=hn2[xb][:, c * 128:(c + 1) * 128],
                                                            identity=idb[:]),
                     reads=["hn%d" % xb, "idb"], writes=["p_tr"])
            hT_key = "hT%d" % hs
            S.op("dve", lambda e, hs=hs, j=j: e.tensor_copy(out=hT[hs][:, :, j * 128:(j + 1) * 128], in_=p_tr[:]),
                 reads=["p_tr"], writes=[hT_key])
            if t + 1 < NTL:
                norm_x(t + 1)
            lhs = lambda c, hs=hs, j=j: hT[hs][:, c, j * 128:(j + 1) * 128]
            if CUT == 1:
                break
            for c in range(8):
                S.op("pe", lambda e, c=c, lhs=lhs: e.matmul(p_vd[:], lhsT=lhs(c), rhs=wb_in[:, c, 1024:1536],
                                                            start=(c == 0), stop=(c == 7)),
                     reads=[hT_key, "wb_in"], writes=["p_vd"])
            vb = t % 2
            S.op("act", lambda e, vb=vb: e.activation(out=vd_t[vb][:, :, 0:64],
                                                      in_=p_vd[:].rearrange("p (h d) -> p h d", d=64), func=AF.Copy),
                 reads=["p_vd"], writes=["vd_t%d" % vb])
            S.dma("sp", lambda e, vb=vb, tok0=tok0: e.dma_start(
                out=D.Vd[tok0:tok0 + 128, :], in_=vd_t[vb][:].rearrange("p h d -> p (h d)")),
                reads=["vd_t%d" % vb], writes=["Vd"])
            if CUT == 2:
                break
            lo = 1536 if own else 1792
            nlat = 1952 - lo
            for c in range(8):
                S.op("pe", lambda e, c=c, lhs=lhs, lo=lo, nlat=nlat: e.matmul(
                    p_lat[:, 0:nlat], lhsT=lhs(c), rhs=wb_in[:, c, lo:1952], start=(c == 0), stop=(c == 7)),
                    reads=[hT_key, "wb_in"], writes=["p_lat"])
            for _ in range(2):
                if flushq:
                    flushq.pop(0)()
            okv = 256 if own else 0
            if CUT == 3:
                break
            S.op("act", lambda e, okv=okv: e.activation(out=junk[:, 0:128], in_=p_lat[:, okv:okv + 128], func=AF.Square,
                                                        accum_out=ssl[1][:]),
                 reads=["p_lat"], writes=["junk", "ss1"])
            rstd_ops(S, ssl[1][:], rsl[1][:], 128, "ss1", "rs1")
            if CUT == 31:
                break
            S.op("dve", lambda e, okv=okv: e.scalar_tensor_tensor(out=ckvn[:], in0=p_lat[:, okv:okv + 128], scalar=rsl[1][:],
                                                                  in1=g_kv[:], op0=ALU.mult, op1=ALU.mult),
                 reads=["p_lat", "rs1", "g_kv"], writes=["ckvn"])
            if CUT == 32:
                break
            S.op("pe", lambda e: e.transpose(out=p_st[:, 0, :], in_=ckvn[:], identity=idb[:]),
                 reads=["ckvn", "idb"], writes=["p_st"])
            S.op("dve", lambda e: e.tensor_copy(out=ckvnT[:], in_=p_st[:, 0, :]), reads=["p_st"], writes=["ckvnT"])
            if CUT == 33:
                break
            for half in range(2):
                S.op("pe", lambda e, half=half: e.matmul(p_kv[:, half * 512:(half + 1) * 512], lhsT=ckvnT[:],
                                                         rhs=wb_ukv[:, half * 512:(half + 1) * 512], start=True, stop=True),
                     reads=["ckvnT", "wb_ukv"], writes=["p_kv"])
            if CUT == 34:
                break
            kvv = p_kv[:].rearrange("p (h x) -> p h x", x=128)
            S.op("act", lambda e, vb=vb, kvv=kvv: e.activation(out=vm_t[vb][:, :, 0:64], in_=kvv[:, :, 64:128], func=AF.Copy),
                 reads=["p_kv"], writes=["vm_t%d" % vb])
            if CUT == 35:
                break
            S.dma("sp", lambda e, vb=vb, tok0=tok0: e.dma_start(
                out=D.Vm[tok0:tok0 + 128, :], in_=vm_t[vb][:].rearrange("p h d -> p (h d)")),
                reads=["vm_t%d" % vb], writes=["Vm"])
            S.op("act", lambda e, kvv=kvv: e.activation(out=km_t[:, :, 0:64], in_=kvv[:, :, 0:64], func=AF.Copy),
                 reads=["p_kv"], writes=["km_t"])
            if CUT == 4:
                break
            S.op("act", lambda e, okv=okv: e.activation(out=kpe[:], in_=p_lat[:, okv + 128:okv + 160], func=AF.Copy),
                 reads=["p_lat"], writes=["kpe"])
            ta = tmpa[:, 0, :]
            tb = tmpb[:, 0, :]
            ck = cosk[:, t, :]
            sk_ = sink[:, t, :]
            S.op("dve", lambda e, ck=ck, ta=ta: e.tensor_tensor(out=ta, in0=kpe[:, 0:16], in1=ck, op=ALU.mult),
                 reads=["kpe", "cosk"], writes=["tmpa"])
            S.op("dve", lambda e, sk_=sk_, tb=tb: e.tensor_tensor(out=tb, in0=kpe[:, 16:32], in1=sk_, op=ALU.mult),
                 reads=["kpe", "sink"], writes=["tmpb"])
            S.op("dve", lambda e, ta=ta, tb=tb: e.tensor_tensor(out=kr[:, 0:16], in0=ta, in1=tb, op=ALU.subtract),
                 reads=["tmpa", "tmpb"], writes=["kr"])
            S.op("dve", lambda e, ck=ck, ta=ta: e.tensor_tensor(out=ta, in0=kpe[:, 16:32], in1=ck, op=ALU.mult),
                 reads=["kpe", "cosk", "kr"], writes=["tmpa"])
            S.op("dve", lambda e, sk_=sk_, tb=tb: e.tensor_tensor(out=tb, in0=kpe[:, 0:16], in1=sk_, op=ALU.mult),
                 reads=["kpe", "sink", "kr"], writes=["tmpb"])
            S.op("dve", lambda e, ta=ta, tb=tb: e.tensor_tensor(out=kr[:, 16:32], in0=ta, in1=tb, op=ALU.add),
                 reads=["tmpa", "tmpb"], writes=["kr"])
            if CUT == 5:
                break
            for h in range(8):
                if h % 2 == 0:
                    S.op("act", lambda e, h=h: e.activation(out=km_t[:, h, 64:96], in_=kr[:], func=AF.Copy),
                         reads=["kr"], writes=["km_pe%d" % h])
                else:
                    S.op("dve", lambda e, h=h: e.tensor_copy(out=km_t[:, h, 64:96], in_=kr[:]),
                         reads=["kr"], writes=["km_pe%d" % h])
            for h in range(8):
                S.op("pe", lambda e, h=h: e.transpose(out=p_mt[0:96, h, :], in_=km_t[:, h, :], identity=idb[:]),
                     reads=["km_t", "km_pe%d" % h, "idb"], writes=["p_mt"])
            S.op("dve", lambda e, hs=hs, j=j: e.tensor_copy(out=kmT[hs][:, :, j * 128:(j + 1) * 128], in_=p_mt[0:96, :, :]),
                 reads=["p_mt"], writes=["kmT%d" % hs])
            if CUT == 6:
                break
            if own:
                S.op("act", lambda e: e.activation(out=junk[:, 0:256], in_=p_lat[:, 0:256], func=AF.Square,
                                                   accum_out=ssl[2][:]), reads=["p_lat"], writes=["junk", "ss2"])
                rstd_ops(S, ssl[2][:], rsl[2][:], 256, "ss2", "rs2")
                S.op("dve", lambda e: e.scalar_tensor_tensor(out=cqn[:], in0=p_lat[:, 0:256], scalar=rsl[2][:], in1=g_q[:],
                                                             op0=ALU.mult, op1=ALU.mult),
                     reads=["p_lat", "rs2", "g_q"], writes=["cqn"])
                for c in range(2):
                    S.op("pe", lambda e, c=c: e.transpose(out=p_st[:, 1 + c, :], in_=cqn[:, c * 128:(c + 1) * 128],
                                                          identity=idb[:]), reads=["cqn", "idb"], writes=["p_st"])
                S.op("dve", lambda e: e.tensor_copy(out=cqnT[:], in_=p_st[:, 1:3, :]), reads=["p_st"], writes=["cqnT"])
                for (c0, c1) in ((0, 512), (512, 768)):
                    for c in range(2):
                        S.op("pe", lambda e, c=c, c0=c0, c1=c1: e.matmul(p_kv[:, c0:c1], lhsT=cqnT[:, c, :],
                                                                         rhs=wb_uq[:, c, c0:c1], start=(c == 0), stop=(c == 1)),
                             reads=["cqnT", "wb_uq"], writes=["p_kv"])
                qv = p_kv[:, 0:768].rearrange("p (h x) -> p h x", x=96)
                S.op("act", lambda e, qv=qv: e.activation(out=qm_t[:, :, 0:64], in_=qv[:, :, 0:64], func=AF.Copy, scale=SC_M),
                     reads=["p_kv"], writes=["qm_t"])
                S.op("act", lambda e, qv=qv: e.activation(out=qpe[:], in_=qv[:, :, 64:96], func=AF.Copy),
                     reads=["p_kv"], writes=["qpe"])
                cq = cosq8[:, t, :, :]
                sq = sinq8[:, t, :, :]
                x1 = qpe[:, :, 0:16]
                x2 = qpe[:, :, 16:32]
                S.op("dve", lambda e, x1=x1, cq=cq: e.tensor_tensor(out=tmpa[:], in0=x1, in1=cq, op=ALU.mult),
                     reads=["qpe", "cosq8"], writes=["tmpa"])
                S.op("dve", lambda e, x2=x2, sq=sq: e.tensor_tensor(out=tmpb[:], in0=x2, in1=sq, op=ALU.mult),
                     reads=["qpe", "sinq8"], writes=["tmpb"])
                S.op("dve", lambda e: e.tensor_tensor(out=qm_t[:, :, 64:80], in0=tmpa[:], in1=tmpb[:], op=ALU.subtract),
                     reads=["tmpa", "tmpb"], writes=["qm_t"])
                S.op("dve", lambda e, x2=x2, cq=cq: e.tensor_tensor(out=tmpa[:], in0=x2, in1=cq, op=ALU.mult),
                     reads=["qpe", "cosq8", "qm_t"], writes=["tmpa"])
                S.op("dve", lambda e, x1=x1, sq=sq: e.tensor_tensor(out=tmpb[:], in0=x1, in1=sq, op=ALU.mult),
                     reads=["qpe", "sinq8", "qm_t"], writes=["tmpb"])
                S.op("dve", lambda e: e.tensor_tensor(out=qm_t[:, :, 80:96], in0=tmpa[:], in1=tmpb[:], op=ALU.add),
                     reads=["tmpa", "tmpb"], writes=["qm_t"])
                for h in range(8):
                    S.op("pe", lambda e, h=h: e.transpose(out=p_mt[0:96, h, :], in_=qm_t[:, h, :], identity=idb[:]),
                         reads=["qm_t", "idb"], writes=["p_mt"])
                S.op("dve", lambda e, hs=hs, j=j: e.tensor_copy(out=qmT[hs][:, :, j * 128:(j + 1) * 128], in_=p_mt[0:96, :, :]),
                     reads=["p_mt"], writes=["qmT%d" % hs])
            if j == 3:
                st0 = s_i * 512
                S.dma("sp", lambda e, hs=hs, st0=st0: e.dma_start(
                    out=D.KmT[:, :, st0:st0 + 512].rearrange("h r t -> r h t"), in_=kmT[hs][:]),
                    reads=["kmT%d" % hs], writes=["KmT"])
                if own:
                    S.dma("sp", lambda e, hs=hs, st0=st0: e.dma_start(
                        out=D.QmT[:, :, st0:st0 + 512].rearrange("h r t -> r h t"), in_=qmT[hs][:]),
                        reads=["qmT%d" % hs], writes=["QmT"])
                jobs = [("k", cc) for cc in range(4)] + ([("q", cc) for cc in range(4)] if own else [])
                for ji, (kind, cc) in enumerate(jobs):
                    def chunk(kind=kind, cc=cc, hs=hs, st0=st0, hT_key=hT_key):
                        col0 = (512 if kind == "k" else 0) + cc * 128
                        for c in range(8):
                            S.op("pe", lambda e, c=c: e.matmul(
                                p_fm[:], lhsT=wb_in[:, c, col0:col0 + 128], rhs=hT[hs][:, c, :], start=(c == 0), stop=(c == 7)),
                                reads=[hT_key, "wb_in"], writes=["p_fm"])
                        fb = fcnt[0] % 4
                        fcnt[0] += 1
                        sc = 1.0 if kind == "k" else SC_D
                        S.op("act", lambda e: e.activation(out=kdT[fb][:], in_=p_fm[:], func=AF.Copy, scale=sc),
                             reads=["p_fm"], writes=["kdT%d" % fb])
                        dst = D.KdTc if kind == "k" else D.QdTc
                        S.dma("act", lambda e: e.dma_start(out=dst[cc, :, st0:st0 + 512], in_=kdT[fb][:]),
                              reads=["kdT%d" % fb], writes=["KQdT"])
                    flushq.append(chunk)
        while flushq:
            flushq.pop(0)()
        S.emit()


def attn_phase(nc, D, kind):
    R = 36 if kind == "d" else 96
    NM = 2 if kind == "d" else 1
    Vsrc = D.Vd if kind == "d" else D.Vm
    NH = int(os.environ.get("ATT_NH", "8"))
    NI = int(os.environ.get("ATT_NI", "8"))
    with contextlib.ExitStack() as st:
        T = lambda name, shape, dt: st.enter_context(nc.sbuf_tensor(name, list(shape), dt))
        PS = lambda name, shape, dt: st.enter_context(nc.psum_tensor(name, list(shape), dt))
        S = Sched(nc)
        idf = T(kind + "_idf", [128, 128], F32)
        idb = T(kind + "_idb", [128, 128], BF16)
        mtmp = T(kind + "_mtmp", [128, 128], F32)
        trib = T(kind + "_trib", [128, 128], BF16)
        flagb = T(kind + "_flagb", [128, 128], BF16)
        V = T(kind + "_V", [128, NT_ALL, 520], BF16)
        if kind == "d":
            KTd = [T(kind + "_KT%d" % b, [128, S_ALL], BF16) for b in range(2)]
            QTd = [T(kind + "_QT%d" % b, [128, S_OWN], BF16) for b in range(2)]
            KT = [[KTd[b][0:36, :], KTd[b][64:100, :]] for b in range(2)]
            QT = [[QTd[b][0:36, :], QTd[b][64:100, :]] for b in range(2)]
        else:
            KT = [[T(kind + "_KT%d%d" % (b, m), [R, S_ALL], BF16)[:, :] for m in range(NM)] for b in range(2)]
            QT = [[T(kind + "_QT%d%d" % (b, m), [R, S_OWN], BF16)[:, :] for m in range(NM)] for b in range(2)]
        pT = [T(kind + "_pT%d" % i, [128, 2, 512], BF16) for i in range(3)]
        accs = T(kind + "_accs", [65, NM, 512], F32)
        o1 = T(kind + "_o1", [128, 64], F32)
        o2 = T(kind + "_o2", [128, 64], F32)
        ob = [T(kind + "_ob%d" % i, [128, 64], BF16) for i in range(2)]
        junk = T(kind + "_junk", [128, 64], BF16)
        r1 = T(kind + "_r1", [128, 1], F32)
        r2 = T(kind + "_r2", [128, 1], F32)
        lam2 = T(kind + "_lam2", [128, 1], F32)
        ss = T(kind + "_ss", [128, 1], F32)
        rs = T(kind + "_rs", [128, 1], F32)
        neglam = T(kind + "_neglam", [128, 1], F32)
        ssacc = T(kind + "_ssacc", [128, NT_OWN], F32)
        lv = [T(kind + "_lv%d" % i, [128, 32], F32) for i in range(4)]
        e1 = T(kind + "_e1", [128, 1], F32)
        e2 = T(kind + "_e2", [128, 1], F32)
        sT = [PS(kind + "_sT%d" % i, [128, 2, 512], F32) for i in range(2)]
        acc = [PS(kind + "_acc%d" % i, [128, 512], F32) for i in range(2)]
        ptr = PS(kind + "_ptr", [128, 2, 128], F32)
        pwarm = PS(kind + "_pwarm", [128, 512], F32)
        WARM = int(os.environ.get("ATT_WARM", "1"))

        S.dma("sp", lambda e: e.dma_start(out=idf[:], in_=D.ident[:, :]), writes=["idf"])
        S.op("dve", lambda e: e.tensor_copy(out=idb[:], in_=idf[:]), reads=["idf"], writes=["idb"])
        S.dma("sp", lambda e: e.dma_start(out=mtmp[:], in_=D.trimask[:, :]), writes=["mtmp"])
        S.op("dve", lambda e: e.tensor_copy(out=trib[:], in_=mtmp[:]), reads=["mtmp"], writes=["trib"])
        S.dma("sp", lambda e: e.dma_start(out=mtmp[:], in_=D.flagmask[:, :]), reads=["trib"], writes=["mtmp"])
        S.op("dve", lambda e: e.tensor_copy(out=flagb[:], in_=mtmp[:]), reads=["mtmp"], writes=["flagb"])
        Vv = Vsrc.rearrange("(t p) c -> p t c", p=128)
        for c in range(8):
            S.dma("sp", lambda e, c=c: e.dma_start(out=V[:, c * 8:(c + 1) * 8, :], in_=Vv[:, c * 8:(c + 1) * 8, :]),
                  writes=["V"])
        if kind == "d":
            srcs = [D.lam_q1, D.lam_k1, D.lam_q2, D.lam_k2]
            for i in range(4):
                S.dma("sp", lambda e, i=i: e.dma_start(out=lv[i][:], in_=srcs[i].partition_broadcast(128)), writes=["lv%d" % i])
            S.op("dve", lambda e: e.tensor_tensor(out=lv[0][:], in0=lv[0][:], in1=lv[1][:], op=ALU.mult),
                 reads=["lv0", "lv1"], writes=["lv0"])
            S.op("dve", lambda e: e.tensor_tensor(out=lv[2][:], in0=lv[2][:], in1=lv[3][:], op=ALU.mult),
                 reads=["lv2", "lv3"], writes=["lv2"])
            S.op("act", lambda e: e.activation(out=lv[1][:], in_=lv[0][:], func=AF.Copy, accum_out=e1[:]),
                 reads=["lv0", "lv1"], writes=["lv1", "e1"])
            S.op("act", lambda e: e.activation(out=lv[3][:], in_=lv[2][:], func=AF.Copy, accum_out=e2[:]),
                 reads=["lv2", "lv3"], writes=["lv3", "e2"])
            S.op("act", lambda e: e.activation(out=e1[:], in_=e1[:], func=AF.Exp), reads=["e1"], writes=["e1"])
            S.op("act", lambda e: e.activation(out=e2[:], in_=e2[:], func=AF.Exp), reads=["e2"], writes=["e2"])
            S.op("dve", lambda e: e.tensor_tensor(out=neglam[:], in0=e2[:], in1=e1[:], op=ALU.subtract),
                 reads=["e1", "e2"], writes=["neglam"])
            S.op("dve", lambda e: e.tensor_single_scalar(out=neglam[:], in_=neglam[:], scalar=-0.2, op=ALU.add),
                 reads=["neglam"], writes=["neglam"])
        else:
            S.op("dve", lambda e: e.memset(ssacc[:], 0.0), writes=["ssacc"])

        if kind == "d":
            for b in range(2):
                S.op("pool", lambda e, b=b: e.memset(KTd[b][:], 0.0), writes=["KT%d0" % b, "KT%d1" % b])
                S.op("pool", lambda e, b=b: e.memset(QTd[b][:], 0.0), writes=["QT%d0" % b, "QT%d1" % b])
        gcount = 0
        acount = 0
        ocnt = [0]
        deferred = []
        for h in range(NH):
            b = h % 2
            for m in range(NM):
                if kind == "d":
                    cc = h // 2
                    g0 = ((h % 2) * 2 + m) * 32
                    for c in range(4):
                        cs = slice(c * 2048, (c + 1) * 2048)
                        S.dma("sp", lambda e, b=b, m=m, cs=cs, cc=cc, g0=g0: e.dma_start(
                            out=KT[b][m][0:32, cs], in_=D.KdTc[cc, g0:g0 + 32, cs]), writes=["KT%d%d" % (b, m)])
                    S.dma("sp", lambda e, b=b, m=m: e.dma_start(out=KT[b][m][32:36, :], in_=D.krow[:, :]),
                          writes=["KT%d%d" % (b, m)])
                    S.dma("sp", lambda e, b=b, m=m, cc=cc, g0=g0: e.dma_start(
                        out=QT[b][m][0:32, :], in_=D.QdTc[cc, g0:g0 + 32, :]), writes=["QT%d%d" % (b, m)])
                    S.dma("sp", lambda e, b=b, m=m, h=h: e.dma_start(out=QT[b][m][32:36, :], in_=D.qrow[h]),
                          writes=["QT%d%d" % (b, m)])
                    continue
                ksrc = D.KmT[h]
                qsrc = D.QmT[h]
                for c in range(4):
                    S.dma("sp", lambda e, b=b, m=m, c=c, ksrc=ksrc: e.dma_start(
                        out=KT[b][m][:, c * 2048:(c + 1) * 2048], in_=ksrc[:, c * 2048:(c + 1) * 2048]),
                        writes=["KT%d%d" % (b, m)])
                S.dma("sp", lambda e, b=b, m=m, qsrc=qsrc: e.dma_start(out=QT[b][m], in_=qsrc[:, :]),
                      writes=["QT%d%d" % (b, m)])
            for I in range(NI):
                if kind == "d":
                    nk = 4 * I + 4
                    pending = None

                    def emit_av2(p, h=h):
                        ktile, c0, pb, first, last = p
                        for m in range(2):
                            S.op("pe", lambda e, m=m: e.matmul(
                                acc[m][0:65, c0:512], lhsT=V[:, ktile, h * 65:(h + 1) * 65], rhs=pT[pb][:, m, c0:512],
                                start=first, stop=last),
                                reads=["V", "pT%d" % pb], writes=["acc%d" % m])
                        if WARM:
                            S.op("pe", lambda e: e.matmul(pwarm[:, c0:512], lhsT=V[:, ktile, 0:128], rhs=pT[pb][:, 0, c0:512],
                                                          start=True, stop=True),
                                 reads=["V", "pT%d" % pb], writes=["pwarm"])

                    for kt in range(nk):
                        a = kt - 4 * I
                        c0 = max(0, a) * 128
                        for g in range(2):
                            sb = gcount % 2
                            pb = gcount % 3
                            gcount += 1
                            koff = kt * 128 if g == 0 else S_OWN + kt * 128
                            ktile = kt if g == 0 else NT_OWN + kt
                            mask = None if a < 0 else (trib if g == 0 else flagb)
                            for m in range(2):
                                S.op("pe", lambda e, m=m, koff=koff, c0=c0, sb=sb, mask=mask, b=b, I=I: e.matmul(
                                    sT[sb][:, m, c0:512], lhsT=KT[b][m][:, koff:koff + 128],
                                    rhs=QT[b][m][:, I * 512 + c0:I * 512 + 512], start=True, stop=(mask is None)),
                                    reads=["KT%d%d" % (b, m), "QT%d%d" % (b, m)], writes=["sT%d" % sb])
                            if mask is not None:
                                for m in range(2):
                                    S.op("pe", lambda e, m=m, c0=c0, sb=sb, mask=mask: e.matmul(
                                        sT[sb][:, m, c0:c0 + 128], lhsT=idb[:], rhs=mask[:], start=False, stop=True),
                                        reads=["idb", "trib", "flagb"], writes=["sT%d" % sb])
                            if pending is not None:
                                emit_av2(pending)
                            S.op("act", lambda e, sb=sb, pb=pb, c0=c0: e.activation(
                                out=pT[pb][:, :, c0:512], in_=sT[sb][:, :, c0:512], func=AF.Exp),
                                reads=["sT%d" % sb], writes=["pT%d" % pb])
                            pending = (ktile, c0, pb, kt == 0 and g == 0, kt == nk - 1 and g == 1)
                            if deferred:
                                deferred.pop(0)()
                    emit_av2(pending)
                    while deferred:
                        deferred.pop(0)()
                    for m in range(2):
                        S.op("dve", lambda e, m=m: e.tensor_copy(out=accs[:, m, :], in_=acc[m][0:65, :]),
                             reads=["acc%d" % m], writes=["accs%d" % m])
                else:
                    for m in range(NM):
                        ab = acount % 2
                        acount += 1
                        nk = 4 * I + 4
                        pending = None

                        def emit_av(p, m=m, h=h):
                            kt, c0, pb, ab_, first, last = p
                            for g in range(2):
                                ktile = kt if g == 0 else NT_OWN + kt
                                S.op("pe", lambda e, g=g, ktile=ktile, c0=c0, pb=pb, ab_=ab_, first=first, last=last: e.matmul(
                                    acc[ab_][0:65, c0:512], lhsT=V[:, ktile, h * 65:(h + 1) * 65], rhs=pT[pb][:, g, c0:512],
                                    start=(first and g == 0), stop=(last and g == 1)),
                                    reads=["V", "pT%d" % pb], writes=["acc%d" % ab_])

                        for kt in range(nk):
                            a = kt - 4 * I
                            c0 = max(0, a) * 128
                            sb = gcount % 2
                            pb = gcount % 3
                            gcount += 1
                            for g in range(2):
                                koff = kt * 128 if g == 0 else S_OWN + kt * 128
                                mask = None if a < 0 else (trib if g == 0 else flagb)
                                S.op("pe", lambda e, g=g, koff=koff, c0=c0, sb=sb, b=b, m=m, I=I, mask=mask: e.matmul(
                                    sT[sb][:, g, c0:512], lhsT=KT[b][m][:, koff:koff + 128],
                                    rhs=QT[b][m][:, I * 512 + c0:I * 512 + 512], start=True, stop=(mask is None)),
                                    reads=["KT%d%d" % (b, m), "QT%d%d" % (b, m)], writes=["sT%d" % sb])
                                if mask is not None:
                                    S.op("pe", lambda e, g=g, c0=c0, sb=sb, mask=mask: e.matmul(
                                        sT[sb][:, g, c0:c0 + 128], lhsT=idb[:], rhs=mask[:], start=False, stop=True),
                                        reads=["idb", "trib", "flagb"], writes=["sT%d" % sb])
                            if pending is not None:
                                emit_av(pending)
                            S.op("act", lambda e, sb=sb, pb=pb, c0=c0: e.activation(
                                out=pT[pb][:, :, c0:512], in_=sT[sb][:, :, c0:512], func=AF.Exp),
                                reads=["sT%d" % sb], writes=["pT%d" % pb])
                            pending = (kt, c0, pb, ab, kt == 0, kt == nk - 1)
                            if deferred:
                                deferred.pop(0)()
                        emit_av(pending)
                        while deferred:
                            deferred.pop(0)()
                        S.op("dve", lambda e, m=m, ab=ab: e.tensor_copy(out=accs[:, m, :], in_=acc[ab][0:65, :]),
                             reads=["acc%d" % ab], writes=["accs%d" % m])
                def epi(qt, I=I, h=h):
                    tile_i = I * 4 + qt
                    tok0 = tile_i * 128
                    for m in range(NM):
                        S.op("pe", lambda e, m=m, qt=qt: e.transpose(out=ptr[:, m, 0:65], in_=accs[:, m, qt * 128:(qt + 1) * 128],
                                                                    identity=idf[0:65, 0:65]),
                             reads=["accs%d" % m, "idf"], writes=["ptr"])
                    obi = ocnt[0] % 2
                    ocnt[0] += 1
                    S.op("dve", lambda e: e.reciprocal(out=r1[:], in_=ptr[:, 0, 64:65]), reads=["ptr"], writes=["r1"])
                    S.op("dve", lambda e: e.tensor_scalar(out=o1[:], in0=ptr[:, 0, 0:64], scalar1=r1[:], scalar2=None,
                                                          op0=ALU.mult), reads=["ptr", "r1"], writes=["o1"])
                    if kind == "d":
                        S.op("dve", lambda e: e.reciprocal(out=r2[:], in_=ptr[:, 1, 64:65]), reads=["ptr"], writes=["r2"])
                        S.op("dve", lambda e: e.tensor_tensor(out=lam2[:], in0=r2[:], in1=neglam[:], op=ALU.mult),
                             reads=["r2", "neglam"], writes=["lam2"])
                        S.op("dve", lambda e: e.scalar_tensor_tensor(out=o2[:], in0=ptr[:, 1, 0:64], scalar=lam2[:], in1=o1[:],
                                                                     op0=ALU.mult, op1=ALU.add),
                             reads=["ptr", "lam2", "o1"], writes=["o2"])
                        S.op("act", lambda e: e.activation(out=junk[:], in_=o2[:], func=AF.Square, accum_out=ss[:]),
                             reads=["o2"], writes=["ajunk", "ss"])
                        rstd_ops(S, ss[:], rs[:], 64, "ss", "rs")
                        S.op("dve", lambda e, obi=obi: e.tensor_scalar(out=ob[obi][:], in0=o2[:], scalar1=rs[:], scalar2=None,
                                                                       op0=ALU.mult), reads=["o2", "rs"], writes=["ob%d" % obi])
                        col0 = h * 64
                    else:
                        S.op("act", lambda e: e.activation(out=junk[:], in_=o1[:], func=AF.Square, accum_out=ss[:]),
                             reads=["o1"], writes=["ajunk", "ss"])
                        S.op("dve", lambda e, tile_i=tile_i: e.tensor_tensor(
                            out=ssacc[:, tile_i:tile_i + 1], in0=ssacc[:, tile_i:tile_i + 1], in1=ss[:], op=ALU.add),
                            reads=["ss", "ssacc"], writes=["ssacc"])
                        S.op("dve", lambda e, obi=obi: e.tensor_copy(out=ob[obi][:], in_=o1[:]),
                             reads=["o1"], writes=["ob%d" % obi])
                        col0 = 512 + h * 64
                    S.dma("pool", lambda e, obi=obi, tok0=tok0, col0=col0: e.dma_start(
                        out=D.AO[tok0:tok0 + 128, col0:col0 + 64], in_=ob[obi][:]),
                        reads=["ob%d" % obi], writes=["AO"])
                for qt in range(4):
                    deferred.append(lambda qt=qt, f=epi: f(qt))
        while deferred:
            deferred.pop(0)()
        if kind == "m":
            S.dma("pool", lambda e: e.dma_start(out=D.SSM[:, :], in_=ssacc[:]), reads=["ssacc"], writes=["SSM"])
        S.emit()


def phase_final_only(nc, D):
    with contextlib.ExitStack() as st:
        T = lambda name, shape, dt: st.enter_context(nc.sbuf_tensor(name, list(shape), dt))
        S = Sched(nc)
        g_fin = T("g_fin", [128, 1024], F32)
        xt = [T("fxt%d" % i, [128, 1024], F32) for i in range(2)]
        ot = [T("fot%d" % i, [128, 1024], F32) for i in range(2)]
        junk = T("fjunk", [128, 1024], BF16)
        ss = T("fss", [128, 1], F32)
        rs = T("frs", [128, 1], F32)
        S.dma("sp", lambda e: e.dma_start(out=g_fin[:], in_=D.norm_final_g.partition_broadcast(128)), writes=["g_fin"])
        for t in range(NT_OWN):
            b = t % 2
            tok0 = t * 128
            S.dma("sp", lambda e, b=b, tok0=tok0: e.dma_start(out=xt[b][:], in_=D.xk[tok0:tok0 + 128, :]), writes=["fxt%d" % b])
            S.op("act", lambda e, b=b: e.activation(out=junk[:], in_=xt[b][:], func=AF.Square, accum_out=ss[:]),
                 reads=["fxt%d" % b], writes=["fjunk", "fss"])
            rstd_ops(S, ss[:], rs[:], 1024, "fss", "frs")
            S.op("dve", lambda e, b=b: e.scalar_tensor_tensor(out=ot[b][:], in0=xt[b][:], scalar=rs[:], in1=g_fin[:],
                                                              op0=ALU.mult, op1=ALU.mult),
                 reads=["fxt%d" % b, "frs", "g_fin"], writes=["fot%d" % b])
            S.dma("pool", lambda e, b=b, tok0=tok0: e.dma_start(out=D.out[tok0:tok0 + 128, :], in_=ot[b][:]),
                  reads=["fot%d" % b], writes=["out"])
        S.emit()


def build(debug=False, upto=99):
    nc = bass.Bass("TRN2", target_bir_lowering=False)
    Sched.reset(nc)
    D = declare_io(nc, debug, upto)
    nc.in_names = D.in_names
    phase1(nc, D)
    if upto >= 2:
        attn_phase(nc, D, "d")
    if upto >= 3:
        attn_phase(nc, D, "m")
    if upto >= 4:
        phase4(nc, D)
    if upto >= 5:
        phase5(nc, D)
    return nc


def host_inputs(inputs):
    x = np.asarray(inputs["x"]); mem = np.asarray(inputs["mem"]); pos = np.asarray(inputs["positions"])
    ident = np.eye(128, dtype=np.float32)
    kk = np.arange(128)[:, None]; qq = np.arange(128)[None, :]
    trimask = np.where(kk <= qq, 0.0, NEG).astype(np.float32)
    sq = lambda a: np.ascontiguousarray(np.asarray(a)[0])
    row = lambda a: np.ascontiguousarray(np.asarray(a)[0].reshape(1, -1))
    shared = {
        "ident": ident, "trimask": trimask,
        "norm_mix_g": row(inputs["norm_mix_g"]), "w_in": sq(inputs["w_in"]),
        "lam_q1": row(inputs["diff_lambda_q1"]), "lam_k1": row(inputs["diff_lambda_k1"]),
        "lam_q2": row(inputs["diff_lambda_q2"]), "lam_k2": row(inputs["diff_lambda_k2"]),
        "diff_out_g": row(inputs["diff_out_g"]), "mla_q_norm_g": row(inputs["mla_q_norm_g"]),
        "w_mla_uq": sq(inputs["w_mla_uq"]), "mla_kv_norm_g": row(inputs["mla_kv_norm_g"]),
        "w_mla_ukv": sq(inputs["w_mla_ukv"]), "mla_out_g": row(inputs["mla_out_g"]),
        "w_out": sq(inputs["w_out"]), "norm_cross_g": row(inputs["norm_cross_g"]),
        "norm_mem_g": row(inputs["norm_mem_g"]), "w_mem_q": sq(inputs["w_mem_q"]),
        "w_mem_kv": sq(inputs["w_mem_kv"]), "w_mem_o": sq(inputs["w_mem_o"]),
        "norm_ffn_g": row(inputs["norm_ffn_g"]), "w_grp": sq(inputs["w_group_router"]),
        "b_grp": row(inputs["b_group_router"]), "w_exr": sq(inputs["w_expert_router"]),
        "b_exr": row(inputs["b_expert_router"]), "w_gate": sq(inputs["w_expert_gate"]),
        "w_up": sq(inputs["w_expert_up"]), "w_down": sq(inputs["w_expert_down"]),
        "norm_final_g": np.ascontiguousarray(np.asarray(inputs["norm_final_g"]).reshape(1, -1)),
    }
    maps = []
    for core in range(8):
        b, p = core // 2, core % 2
        xb = x[b].reshape(NT_ALL, 128, 1024)
        order = list(range(p, NT_ALL, 2)) + list(range(1 - p, NT_ALL, 2))
        xk = np.ascontiguousarray(xb[order].reshape(S_ALL, 1024))
        pk = pos[b].reshape(NT_ALL, 128)[order]
        m = dict(shared)
        m["xk"] = xk
        m["postok"] = np.ascontiguousarray(pk.T.astype(np.int32))
        m["mem"] = np.ascontiguousarray(mem[b])
        m["flagmask"] = np.full((128, 128), 0.0 if p == 1 else NEG, np.float32)
        maps.append(m)
    return maps


def kernel(**inputs):
    nc = build()
    maps = host_inputs(inputs)
    res = run_bass_kernel_spmd(nc, maps, core_ids=list(range(8)))
    out = np.zeros((4, S_ALL, 1024), np.float32)
    for core in range(8):
        b, p = core // 2, core % 2
        o = np.asarray(res.results[core]["out"]).reshape(NT_OWN, 128, 1024)
        out[b].reshape(NT_ALL, 128, 1024)[p::2] = o
    return out


def phase4(nc, D):
    NTL = int(os.environ.get("P4NT", str(NT_OWN)))
    with contextlib.ExitStack() as st:
        T = lambda name, shape, dt: st.enter_context(nc.sbuf_tensor("p4_" + name, list(shape), dt))
        PS = lambda name, shape, dt: st.enter_context(nc.psum_tensor("p4_" + name, list(shape), dt))
        S = Sched(nc)
        idf = T("idf", [128, 128], F32)
        idb = T("idb", [128, 128], BF16)
        onesb = T("onesb", [128, 128], BF16)
        wb_out = T("wb_out", [128, 8, 1024], BF16)
        wb_q = T("wb_q", [128, 8, 512], BF16)
        wb_kv = T("wb_kv", [128, 8, 1024], BF16)
        wb_o = T("wb_o", [128, 4, 1024], BF16)
        wr = T("wr", [128, 8, 36], F32)
        br = T("br", [128, 36], F32)
        g_cross = T("g_cross", [128, 1024], F32)
        g_mem = T("g_mem", [128, 1024], F32)
        g_ffn = T("g_ffn", [128, 1024], F32)
        gvs = [T("gv%d" % c, [128, 1], F32) for c in range(8)]
        ssm = T("ssm", [128, NT_OWN], F32)
        memt = T("memt", [128, 1024], F32)
        memn = T("memn", [128, 1024], BF16)
        memT = T("memT", [128, 8, 256], BF16)
        KmemT = T("KmemT", [128, 4, 256], BF16)
        Vmem = T("Vmem", [128, 2, 512], BF16)
        WR = T("WR", [128, NT_OWN, 32], F32)
        ao = [T("ao%d" % i, [128, 1024], BF16) for i in range(2)]
        aoT = T("aoT", [128, 8, 128], BF16)
        xt = [T("xt%d" % i, [128, 1024], F32) for i in range(2)]
        x1 = T("x1", [128, 1024], F32)
        x2 = [T("x2%d" % i, [128, 1024], F32) for i in range(2)]
        junk = T("junk", [128, 1024], BF16)
        ss = T("ss", [128, 1], F32)
        rs = T("rs", [128, 1], F32)
        rm = T("rm", [128, 1], F32)
        hb = T("hb", [128, 1024], BF16)
        hf = T("hf", [128, 1024], F32)
        hT = T("hT", [128, 8, 128], BF16)
        h3T = [T("h3T%d" % i, [128, 8, 128], BF16) for i in range(2)]
        hTf = T("hTf", [128, 8, 128], F32)
        qT = T("qT", [128, 4, 128], BF16)
        pxT = T("pxT", [128, 8, 128], BF16)
        rl = T("rl", [128, 512], F32)
        oxn = T("oxn", [128, 4, 128], BF16)
        lg = T("lg", [128, 36], F32)
        gmax = T("gmax", [128, 1], F32)
        gsum = T("gsum", [128, 1], F32)
        ggate = T("ggate", [128, 1], F32)
        gexp = T("gexp", [128, 4], F32)
        oh = T("oh", [128, 4], F32)
        pen = T("pen", [128, 4], F32)
        pen32 = T("pen32", [128, 4, 8], F32)
        elm = T("elm", [128, 32], F32)
        elm2 = T("elm2", [128, 32], F32)
        eq1 = T("eq1", [128, 32], F32)
        eq2 = T("eq2", [128, 32], F32)
        m1 = T("m1", [128, 1], F32)
        m2 = T("m2", [128, 1], F32)
        w1 = T("w1", [128, 1], F32)
        w2 = T("w2", [128, 1], F32)

        pA = PS("pA", [128, 1024], F32)
        pB = PS("pB", [128, 1024], F32)
        pT8 = PS("pT8", [128, 8, 128], BF16)
        pQ = PS("pQ", [128, 4, 128], F32)
        pL = PS("pL", [128, 4, 128], F32)
        pR = PS("pR", [128, 512], F32)

        S.dma("sp", lambda e: e.dma_start(out=idf[:], in_=D.ident[:, :]), writes=["idf"])
        S.op("dve", lambda e: e.tensor_copy(out=idb[:], in_=idf[:]), reads=["idf"], writes=["idb"])
        S.op("dve", lambda e: e.memset(onesb[:], 1.0), writes=["onesb"])
        S.op("dve", lambda e: e.memset(WR[:], 0.0), writes=["WR"])
        S.dma("sp", lambda e: e.dma_start(out=g_cross[:], in_=D.norm_cross_g.partition_broadcast(128)), writes=["g_cross"])
        S.dma("sp", lambda e: e.dma_start(out=g_mem[:], in_=D.norm_mem_g.partition_broadcast(128)), writes=["g_mem"])
        S.dma("sp", lambda e: e.dma_start(out=g_ffn[:], in_=D.norm_ffn_g.partition_broadcast(128)), writes=["g_ffn"])
        S.dma("sp", lambda e: e.dma_start(out=ssm[:], in_=D.SSM[:, :]), writes=["ssm"])
        S.dma("sp", lambda e: e.dma_start(out=wr[:, :, 0:4], in_=D.w_grp.rearrange("(c p) g -> p c g", p=128)), writes=["wr"])
        S.dma("sp", lambda e: e.dma_start(out=wr[:, :, 4:36], in_=D.w_exr.rearrange("(c p) g -> p c g", p=128)), writes=["wr"])
        S.dma("sp", lambda e: e.dma_start(out=br[:, 0:4], in_=D.b_grp.partition_broadcast(128)), writes=["br"])
        S.dma("sp", lambda e: e.dma_start(out=br[:, 4:36], in_=D.b_exr.partition_broadcast(128)), writes=["br"])
        dg = D.diff_out_g.rearrange("o d -> d o")
        for c in range(4):
            S.dma("sp", lambda e, c=c: e.dma_start(out=gvs[c][0:64, :], in_=dg), writes=["gv%d" % c])
            S.dma("sp", lambda e, c=c: e.dma_start(out=gvs[c][64:128, :], in_=dg), writes=["gv%d" % c])
            S.op("dve", lambda e, c=c: e.tensor_single_scalar(out=gvs[c][:], in_=gvs[c][:], scalar=0.8, op=ALU.mult),
                 reads=["gv%d" % c], writes=["gv%d" % c])
        mg = D.mla_out_g.rearrange("o (c p) -> c p o", p=128)
        for c in range(4):
            S.dma("sp", lambda e, c=c: e.dma_start(out=gvs[4 + c][:], in_=mg[c]), writes=["gv%d" % (4 + c)])
        w_out_v = D.w_out.rearrange("(c p) n -> p c n", p=128)
        for c in range(8):
            S.dma("pool", lambda e, c=c: e.dma_start(out=wb_out[:, c, :], in_=w_out_v[:, c, :]), writes=["wb_out%d" % c])
            S.op("dve", lambda e, c=c: e.tensor_scalar(out=wb_out[:, c, :], in0=wb_out[:, c, :], scalar1=gvs[c][:], scalar2=None,
                                                       op0=ALU.mult), reads=["wb_out%d" % c, "gv%d" % c], writes=["wb_out%d" % c])
        S.dma("pool", lambda e: e.dma_start(out=wb_q[:], in_=D.w_mem_q.rearrange("(c p) n -> p c n", p=128)), writes=["wb_q"])
        w_kv_v = D.w_mem_kv.rearrange("(c p) n -> p c n", p=128)
        for c in range(8):
            S.dma("pool", lambda e, c=c: e.dma_start(out=wb_kv[:, c, :], in_=w_kv_v[:, c, :]), writes=["wb_kv"])
        S.dma("pool", lambda e: e.dma_start(out=wb_o[:], in_=D.w_mem_o.rearrange("(c p) n -> p c n", p=128)), writes=["wb_o"])
        wb_out_keys = ["wb_out%d" % c for c in range(8)]

        def transposes8(src, src_key, dst, dst_key, eng="dve"):
            for c in range(8):
                S.op("pe", lambda e, c=c: e.transpose(out=pT8[:, c, :], in_=src[:, c * 128:(c + 1) * 128], identity=idb[:]),
                     reads=[src_key, "idb"], writes=["pT8"])
            if eng == "dve":
                S.op("dve", lambda e: e.tensor_copy(out=dst, in_=pT8[:]), reads=["pT8"], writes=[dst_key])
            else:
                S.op("act", lambda e: e.activation(out=dst, in_=pT8[:], func=AF.Copy), reads=["pT8"], writes=[dst_key])

        def norm_tile(src, src_key, g, g_key, outs):
            S.op("act", lambda e: e.activation(out=junk[:], in_=src, func=AF.Square, accum_out=ss[:]),
                 reads=[src_key], writes=["junk", "ss"])
            rstd_ops(S, ss[:], rs[:], 1024, "ss", "rs")
            for (oap, okey) in outs:
                S.op("dve", lambda e, oap=oap: e.scalar_tensor_tensor(out=oap, in0=src, scalar=rs[:], in1=g[:],
                                                                      op0=ALU.mult, op1=ALU.mult),
                     reads=[src_key, "rs", g_key], writes=[okey])

        for mc in range(2):
            S.dma("sp", lambda e, mc=mc: e.dma_start(out=memt[:], in_=D.mem[mc * 128:(mc + 1) * 128, :]), writes=["memt"])
            norm_tile(memt[:], "memt", g_mem, "g_mem", [(memn[:], "memn")])
            transposes8(memn, "memn", memT[:, :, mc * 128:(mc + 1) * 128], "memT")
        for h in range(4):
            for c in range(8):
                S.op("pe", lambda e, h=h, c=c: e.matmul(pL[:, 0:2, :].rearrange("p a b -> p (a b)"),
                                                        lhsT=wb_kv[:, c, h * 128:(h + 1) * 128], rhs=memT[:, c, :],
                                                        start=(c == 0), stop=(c == 7)),
                     reads=["wb_kv", "memT"], writes=["pL"])
            S.op("dve", lambda e, h=h: e.tensor_copy(out=KmemT[:, h, :], in_=pL[:, 0:2, :].rearrange("p a b -> p (a b)")),
                 reads=["pL"], writes=["KmemT"])
        for mc in range(2):
            for c in range(8):
                S.op("pe", lambda e, mc=mc, c=c: e.matmul(pR[:], lhsT=memT[:, c, mc * 128:(mc + 1) * 128],
                                                          rhs=wb_kv[:, c, 512:1024], start=(c == 0), stop=(c == 7)),
                     reads=["wb_kv", "memT"], writes=["pR"])
            S.op("dve", lambda e, mc=mc: e.tensor_copy(out=Vmem[:, mc, :], in_=pR[:]), reads=["pR"], writes=["Vmem"])

        for t in range(NTL):
            b = t % 2
            tok0 = t * 128
            S.dma("sp", lambda e, b=b, tok0=tok0: e.dma_start(out=ao[b][:], in_=D.AO[tok0:tok0 + 128, :]), writes=["ao%d" % b])
            S.dma("sp", lambda e, b=b, tok0=tok0: e.dma_start(out=xt[b][:], in_=D.xk[tok0:tok0 + 128, :]), writes=["xt%d" % b])
            transposes8(ao[b], "ao%d" % b, aoT[:], "aoT")
            for half in range(2):
                for c in range(4):
                    S.op("pe", lambda e, half=half, c=c: e.matmul(pA[:, half * 512:(half + 1) * 512], lhsT=aoT[:, c, :],
                                                                  rhs=wb_out[:, c, half * 512:(half + 1) * 512],
                                                                  start=(c == 0), stop=(c == 3)),
                         reads=["aoT"] + wb_out_keys, writes=["pA"])
                for c in range(4, 8):
                    S.op("pe", lambda e, half=half, c=c: e.matmul(pB[:, half * 512:(half + 1) * 512], lhsT=aoT[:, c, :],
                                                                  rhs=wb_out[:, c, half * 512:(half + 1) * 512],
                                                                  start=(c == 4), stop=(c == 7)),
                         reads=["aoT"] + wb_out_keys, writes=["pB"])
            S.op("dve", lambda e, t=t: e.tensor_copy(out=ss[:], in_=ssm[:, t:t + 1]), reads=["ssm"], writes=["ss"])
            S.op("act", lambda e: e.activation(out=rm[:], in_=ss[:], func=AF.Ln, scale=1.0 / 512, bias=EPS),
                 reads=["ss"], writes=["rm"])
            S.op("act", lambda e: e.activation(out=rm[:], in_=rm[:], func=AF.Exp, scale=-0.5), reads=["rm"], writes=["rm"])
            for half in range(2):
                sl = slice(half * 512, (half + 1) * 512)
                S.op("dve", lambda e, sl=sl, b=b: e.tensor_tensor(out=x1[:, sl], in0=pA[:, sl], in1=xt[b][:, sl], op=ALU.add),
                     reads=["pA", "xt%d" % b], writes=["x1"])
                S.op("dve", lambda e, sl=sl: e.scalar_tensor_tensor(out=x1[:, sl], in0=pB[:, sl], scalar=rm[:], in1=x1[:, sl],
                                                                    op0=ALU.mult, op1=ALU.add),
                     reads=["pB", "rm", "x1"], writes=["x1"])
            norm_tile(x1[:], "x1", g_cross, "g_cross", [(hb[:], "hb")])
            transposes8(hb, "hb", hT[:], "hT")
            for h in range(4):
                for c in range(8):
                    S.op("pe", lambda e, h=h, c=c: e.matmul(pQ[:, h, :], lhsT=wb_q[:, c, h * 128:(h + 1) * 128], rhs=hT[:, c, :],
                                                            start=(c == 0), stop=(c == 7)),
                         reads=["wb_q", "hT"], writes=["pQ"])
            S.op("act", lambda e: e.activation(out=qT[:], in_=pQ[:], func=AF.Copy, scale=SC_X), reads=["pQ"], writes=["qT"])
            pAv = pA[:].rearrange("p (a b) -> p a b", b=128)
            for h in range(4):
                for mc in range(2):
                    S.op("pe", lambda e, h=h, mc=mc: e.matmul(pAv[:, h * 2 + mc, :], lhsT=KmemT[:, h, mc * 128:(mc + 1) * 128],
                                                              rhs=qT[:, h, :], start=True, stop=True),
                         reads=["KmemT", "qT"], writes=["pA"])
            S.op("act", lambda e: e.activation(out=pxT[:].rearrange("p a b -> p (a b)"), in_=pA[:], func=AF.Exp),
                 reads=["pA"], writes=["pxT"])
            for h in range(4):
                for mc in range(2):
                    S.op("pe", lambda e, h=h, mc=mc: e.matmul(pQ[:, h, :], lhsT=Vmem[:, mc, h * 128:(h + 1) * 128],
                                                              rhs=pxT[:, h * 2 + mc, :], start=(mc == 0), stop=(mc == 1)),
                         reads=["Vmem", "pxT"], writes=["pQ"])
                for mc in range(2):
                    S.op("pe", lambda e, h=h, mc=mc: e.matmul(pL[:, h, :], lhsT=onesb[:], rhs=pxT[:, h * 2 + mc, :],
                                                              start=(mc == 0), stop=(mc == 1)),
                         reads=["onesb", "pxT"], writes=["pL"])
            S.op("dve", lambda e: e.reciprocal(out=rl[:], in_=pL[:].rearrange("p a b -> p (a b)")), reads=["pL"], writes=["rl"])
            S.op("dve", lambda e: e.tensor_tensor(out=oxn[:].rearrange("p a b -> p (a b)"),
                                                  in0=pQ[:].rearrange("p a b -> p (a b)"), in1=rl[:], op=ALU.mult),
                 reads=["pQ", "rl"], writes=["oxn"])
            for half in range(2):
                for h in range(4):
                    S.op("pe", lambda e, half=half, h=h: e.matmul(pB[:, half * 512:(half + 1) * 512], lhsT=oxn[:, h, :],
                                                                  rhs=wb_o[:, h, half * 512:(half + 1) * 512],
                                                                  start=(h == 0), stop=(h == 3)),
                         reads=["oxn", "wb_o"], writes=["pB"])
            for half in range(2):
                sl = slice(half * 512, (half + 1) * 512)
                S.op("dve", lambda e, sl=sl, b=b: e.tensor_tensor(out=x2[b][:, sl], in0=pB[:, sl], in1=x1[:, sl], op=ALU.add),
                     reads=["pB", "x1"], writes=["x2%d" % b])
            S.dma("pool", lambda e, b=b, tok0=tok0: e.dma_start(out=D.X2[tok0:tok0 + 128, :], in_=x2[b][:]),
                  reads=["x2%d" % b], writes=["X2"])
            norm_tile(x2[b][:], "x2%d" % b, g_ffn, "g_ffn", [(hf[:], "hf"), (hb[:], "hb")])
            transposes8(hb, "hb", h3T[b][:], "h3T%d" % b, eng="act")
            S.dma("pool", lambda e, b=b, tok0=tok0: e.dma_start(out=D.H3T[:, :, tok0:tok0 + 128], in_=h3T[b][:]),
                  reads=["h3T%d" % b], writes=["H3T"])
            pBv = pB[:].rearrange("p (a b) -> p a b", b=128)
            for c in range(8):
                S.op("pe", lambda e, c=c: e.transpose(out=pBv[:, c, :], in_=hf[:, c * 128:(c + 1) * 128], identity=idf[:]),
                     reads=["hf", "idf"], writes=["pB"])
            S.op("act", lambda e: e.activation(out=hTf[:].rearrange("p a b -> p (a b)"), in_=pB[:], func=AF.Copy),
                 reads=["pB"], writes=["hTf"])
            for c in range(8):
                S.op("pe", lambda e, c=c: e.matmul(pR[:, 0:36], lhsT=hTf[:, c, :], rhs=wr[:, c, :], start=(c == 0), stop=(c == 7)),
                     reads=["hTf", "wr"], writes=["pR"])
            S.op("dve", lambda e: e.tensor_tensor(out=lg[:], in0=pR[:, 0:36], in1=br[:], op=ALU.add),
                 reads=["pR", "br"], writes=["lg"])
            S.op("dve", lambda e: e.reduce_max(out=gmax[:], in_=lg[:, 0:4], axis=mybir.AxisListType.X), reads=["lg"], writes=["gmax"])
            S.op("dve", lambda e: e.tensor_scalar(out=oh[:], in0=lg[:, 0:4], scalar1=gmax[:], scalar2=None, op0=ALU.is_equal),
                 reads=["lg", "gmax"], writes=["oh"])
            S.op("dve", lambda e: e.tensor_scalar(out=gexp[:], in0=lg[:, 0:4], scalar1=gmax[:], scalar2=None, op0=ALU.subtract),
                 reads=["lg", "gmax"], writes=["gexp"])
            S.op("act", lambda e: e.activation(out=gexp[:], in_=gexp[:], func=AF.Exp, accum_out=gsum[:]),
                 reads=["gexp"], writes=["gexp", "gsum"])
            S.op("dve", lambda e: e.reciprocal(out=ggate[:], in_=gsum[:]), reads=["gsum"], writes=["ggate"])
            S.op("dve", lambda e: e.tensor_scalar(out=pen[:], in0=oh[:], scalar1=-1.0, scalar2=1e30, op0=ALU.add, op1=ALU.mult),
                 reads=["oh"], writes=["pen"])
            for j in range(8):
                S.op("dve", lambda e, j=j: e.tensor_copy(out=pen32[:, :, j], in_=pen[:]), reads=["pen"], writes=["pen32"])
            S.op("dve", lambda e: e.tensor_tensor(out=elm[:], in0=lg[:, 4:36], in1=pen32[:].rearrange("p a b -> p (a b)"), op=ALU.add),
                 reads=["lg", "pen32"], writes=["elm"])
            S.op("dve", lambda e: e.reduce_max(out=m1[:], in_=elm[:], axis=mybir.AxisListType.X), reads=["elm"], writes=["m1"])
            S.op("dve", lambda e: e.tensor_scalar(out=eq1[:], in0=elm[:], scalar1=m1[:], scalar2=None, op0=ALU.is_equal),
                 reads=["elm", "m1"], writes=["eq1"])
            S.op("dve", lambda e: e.scalar_tensor_tensor(out=elm2[:], in0=eq1[:], scalar=-1e30, in1=elm[:], op0=ALU.mult, op1=ALU.add),
                 reads=["eq1", "elm"], writes=["elm2"])
            S.op("dve", lambda e: e.reduce_max(out=m2[:], in_=elm2[:], axis=mybir.AxisListType.X), reads=["elm2"], writes=["m2"])
            S.op("dve", lambda e: e.tensor_scalar(out=eq2[:], in0=elm2[:], scalar1=m2[:], scalar2=None, op0=ALU.is_equal),
                 reads=["elm2", "m2"], writes=["eq2"])
            S.op("dve", lambda e: e.tensor_tensor(out=w1[:], in0=m2[:], in1=m1[:], op=ALU.subtract), reads=["m1", "m2"], writes=["w1"])
            S.op("act", lambda e: e.activation(out=w1[:], in_=w1[:], func=AF.Exp), reads=["w1"], writes=["w1"])
            S.op("dve", lambda e: e.tensor_single_scalar(out=w1[:], in_=w1[:], scalar=1.0, op=ALU.add), reads=["w1"], writes=["w1"])
            S.op("dve", lambda e: e.reciprocal(out=w1[:], in_=w1[:]), reads=["w1"], writes=["w1"])
            S.op("dve", lambda e: e.tensor_scalar(out=w2[:], in0=w1[:], scalar1=-1.0, scalar2=1.0, op0=ALU.mult, op1=ALU.add),
                 reads=["w1"], writes=["w2"])
            S.op("dve", lambda e: e.tensor_tensor(out=w1[:], in0=w1[:], in1=ggate[:], op=ALU.mult), reads=["w1", "ggate"], writes=["w1"])
            S.op("dve", lambda e: e.tensor_tensor(out=w2[:], in0=w2[:], in1=ggate[:], op=ALU.mult), reads=["w2", "ggate"], writes=["w2"])
            S.op("dve", lambda e: e.tensor_scalar(out=eq1[:], in0=eq1[:], scalar1=w1[:], scalar2=None, op0=ALU.mult),
                 reads=["eq1", "w1"], writes=["eq1"])
            S.op("dve", lambda e, t=t: e.scalar_tensor_tensor(out=WR[:, t, :], in0=eq2[:], scalar=w2[:], in1=eq1[:],
                                                              op0=ALU.mult, op1=ALU.add),
                 reads=["eq2", "w2", "eq1"], writes=["WR"])
        S.dma("pool", lambda e: e.dma_start(out=D.WRD[:, :, :], in_=WR[:]), reads=["WR"], writes=["WRD"])
        S.emit()


def phase5(nc, D):
    NE = int(os.environ.get("P5NE", "32"))
    NHALF = int(os.environ.get("P5NH", "2"))
    TPH = int(os.environ.get("P5TPH", "16"))
    with contextlib.ExitStack() as st:
        T = lambda name, shape, dt: st.enter_context(nc.sbuf_tensor("p5_" + name, list(shape), dt))
        PS = lambda name, shape, dt: st.enter_context(nc.psum_tensor("p5_" + name, list(shape), dt))
        S = Sched(nc)
        idb = T("idb", [128, 128], BF16)
        idf = T("idf", [128, 128], F32)
        hT = T("hT", [128, 8, S_OWN], BF16)
        WR = T("WR", [128, NT_OWN, 32], F32)
        yacc = T("yacc", [128, 16, 1024], F32)
        wgu = [T("wgu%d" % i, [128, 8, 512], BF16) for i in range(2)]
        wd = [T("wd%d" % i, [128, 2, 1024], BF16) for i in range(2)]
        sg = [T("sg%d" % i, [128, 256], F32) for i in range(2)]
        hid = [T("hid%d" % i, [128, 256], BF16) for i in range(2)]
        hidT = [T("hidT%d" % i, [128, 2, 128], BF16) for i in range(2)]
        wsc = [T("wsc%d" % i, [128, 1], F32) for i in range(2)]
        g_fin = T("g_fin", [128, 1024], F32)
        xt = [T("xt%d" % i, [128, 1024], F32) for i in range(2)]
        junk = T("junk", [128, 1024], BF16)
        ss = T("ss", [128, 1], F32)
        rs = T("rs", [128, 1], F32)
        pg = [PS("pg%d" % i, [128, 512], F32) for i in range(2)]
        pt = [PS("pt%d" % i, [128, 8, 128], BF16) for i in range(2)]
        pdn = [PS("pdn%d" % i, [128, 1024], F32) for i in range(2)]

        S.dma("sp", lambda e: e.dma_start(out=idf[:], in_=D.ident[:, :]), writes=["idf"])
        S.op("dve", lambda e: e.tensor_copy(out=idb[:], in_=idf[:]), reads=["idf"], writes=["idb"])
        S.dma("sp", lambda e: e.dma_start(out=g_fin[:], in_=D.norm_final_g.partition_broadcast(128)), writes=["g_fin"])
        S.dma("sp", lambda e: e.dma_start(out=WR[:], in_=D.WRD[:, :, :]), writes=["WR"])
        NHT = int(os.environ.get("P5HT", str(S_OWN)))
        for c in range(8):
            S.dma("sp", lambda e, c=c: e.dma_start(out=hT[:, c, 0:NHT], in_=D.H3T[:, c, 0:NHT]), writes=["hT"])
        def load_w(ex, wb):
            S.dma("pool", lambda e: e.dma_start(
                out=wgu[wb][:, :, 0:256], in_=D.w_gate[ex].rearrange("(c p) f -> p c f", p=128)), writes=["wgu%d" % wb])
            S.dma("pool", lambda e: e.dma_start(
                out=wgu[wb][:, :, 256:512], in_=D.w_up[ex].rearrange("(c p) f -> p c f", p=128)), writes=["wgu%d" % wb])
            S.dma("pool", lambda e: e.dma_start(
                out=wd[wb][:], in_=D.w_down[ex].rearrange("(c p) n -> p c n", p=128)), writes=["wd%d" % wb])

        def stage1a(u):
            ex, wb, lt, t, gb = u
            for c in range(8):
                S.op("pe", lambda e, c=c: e.matmul(
                    pg[gb][:], lhsT=hT[:, c, t * 128:(t + 1) * 128], rhs=wgu[wb][:, c, :], start=(c == 0), stop=(c == 7)),
                    reads=["hT", "wgu%d" % wb], writes=["pg%d" % gb])

        def stage1b(u):
            ex, wb, lt, t, gb = u
            S.op("act", lambda e: e.activation(out=sg[gb][:], in_=pg[gb][:, 0:256], func=AF.Silu),
                 reads=["pg%d" % gb], writes=["sg%d" % gb])
            S.op("act", lambda e: e.activation(out=wsc[gb][:], in_=WR[:, t, ex:ex + 1], func=AF.Copy),
                 reads=["WR"], writes=["wsc%d" % gb])
            S.op("dve", lambda e: e.scalar_tensor_tensor(out=hid[gb][:], in0=pg[gb][:, 256:512], scalar=wsc[gb][:],
                                                         in1=sg[gb][:], op0=ALU.mult, op1=ALU.mult),
                 reads=["pg%d" % gb, "wsc%d" % gb, "sg%d" % gb], writes=["hid%d" % gb])

        def stage2a(u):
            ex, wb, lt, t, gb = u
            for fc in range(2):
                S.op("pe", lambda e, fc=fc: e.transpose(out=pt[gb][:, fc, :], in_=hid[gb][:, fc * 128:(fc + 1) * 128],
                                                        identity=idb[:]),
                     reads=["hid%d" % gb, "idb"], writes=["pt%d" % gb])
            S.op("act", lambda e: e.activation(out=hidT[gb][:], in_=pt[gb][:, 0:2, :], func=AF.Copy),
                 reads=["pt%d" % gb], writes=["hidT%d" % gb])

        def stage2b(u):
            ex, wb, lt, t, gb = u
            for h2 in range(2):
                for fc in range(2):
                    S.op("pe", lambda e, h2=h2, fc=fc: e.matmul(
                        pdn[gb][:, h2 * 512:(h2 + 1) * 512], lhsT=hidT[gb][:, fc, :],
                        rhs=wd[wb][:, fc, h2 * 512:(h2 + 1) * 512], start=(fc == 0), stop=(fc == 1)),
                        reads=["hidT%d" % gb, "wd%d" % wb], writes=["pdn%d" % gb])
            for h2 in range(2):
                sl = slice(h2 * 512, (h2 + 1) * 512)
                if ex == 0:
                    S.op("dve", lambda e, sl=sl: e.tensor_copy(out=yacc[:, lt, sl], in_=pdn[gb][:, sl]),
                         reads=["pdn%d" % gb], writes=["yacc%d" % lt])
                else:
                    S.op("dve", lambda e, sl=sl: e.tensor_tensor(out=yacc[:, lt, sl], in0=pdn[gb][:, sl],
                                                                 in1=yacc[:, lt, sl], op=ALU.add),
                         reads=["pdn%d" % gb, "yacc%d" % lt], writes=["yacc%d" % lt])

        cnt = 0
        wcount = 0
        for half in range(NHALF):
            load_w(0, wcount % 2)
            p1 = None
            p2 = None
            for ex in range(NE):
                wb = wcount % 2
                wcount += 1
                for lt in range(TPH):
                    t = half * 16 + lt
                    u = (ex, wb, lt, t, cnt % 2)
                    cnt += 1
                    stage1a(u)
                    if p1 is not None:
                        stage2a(p1)
                    stage1b(u)
                    if p2 is not None:
                        stage2b(p2)
                    p2 = p1
                    p1 = u
                    if lt == 1 and ex + 1 < NE:
                        load_w(ex + 1, wcount % 2)
            stage2a(p1)
            if p2 is not None:
                stage2b(p2)
            stage2b(p1)
            for lt in range(TPH):
                t = half * 16 + lt
                tok0 = t * 128
                b = lt % 2
                S.dma("sp", lambda e, b=b, tok0=tok0: e.dma_start(out=xt[b][:], in_=D.X2[tok0:tok0 + 128, :]), writes=["xt%d" % b])
                S.op("pool", lambda e, b=b, lt=lt: e.tensor_tensor(out=xt[b][:], in0=xt[b][:], in1=yacc[:, lt, :], op=ALU.add),
                     reads=["xt%d" % b, "yacc%d" % lt], writes=["xt%d" % b])
                S.op("act", lambda e, b=b: e.activation(out=junk[:], in_=xt[b][:], func=AF.Square, accum_out=ss[:]),
                     reads=["xt%d" % b], writes=["junk", "ss"])
                rstd_ops(S, ss[:], rs[:], 1024, "ss", "rs")
                S.op("dve", lambda e, b=b: e.scalar_tensor_tensor(out=xt[b][:], in0=xt[b][:], scalar=rs[:], in1=g_fin[:],
                                                                  op0=ALU.mult, op1=ALU.mult),
                     reads=["xt%d" % b, "rs", "g_fin"], writes=["xt%d" % b])
                S.dma("sp", lambda e, b=b, tok0=tok0: e.dma_start(out=D.out[tok0:tok0 + 128, :], in_=xt[b][:]),
                      reads=["xt%d" % b], writes=["out"])
        S.emit()
```

```python
import contextlib
import os
import numpy as np
import ml_dtypes
import concourse.bass as bass
import concourse.mybir as mybir
from concourse.bass_utils import run_bass_kernel_spmd

F32 = mybir.dt.float32
BF16 = mybir.dt.bfloat16
I32 = mybir.dt.int32
AF = mybir.ActivationFunctionType
ALU = mybir.AluOpType

NEG = -30000.0
EPS = 1e-6
S_ALL = 8192
S_OWN = 4096
NT_ALL = 64
NT_OWN = 32
PI = float(np.pi)
TWO_PI = float(2 * np.pi)
SLOPES = [2.0 ** (-(h + 1)) for h in range(8)]
INV_FREQ = [float(np.float32(10000.0) ** np.float32(-(2 * j) / 32.0)) for j in range(16)]
SC_D = float(32 ** -0.5)
SC_M = float(96 ** -0.5)
SC_X = float(128 ** -0.5)

ENGS = ("pe", "act", "dve", "pool", "sp")


class Sched:
    csem = None
    cbase = None
    pools = None

    @classmethod
    def reset(cls, nc):
        cls.csem = {e: nc.alloc_semaphore(name="c_" + e) for e in ENGS}
        cls.cbase = {e: 0 for e in ENGS}
        cls.pools = {"sw": [], "hw": []}
        cls.nalloc = 0

    def __init__(self, nc, same_engine_sync=True):
        self.nc = nc
        self.same = same_engine_sync
        self.ops = {e: [] for e in ENGS}
        self.count = dict(Sched.cbase)
        self.last_w = {}
        self.readers = {}
        self.dma_keys = {}

    def _deps(self, reads, writes):
        deps = []
        for k in reads:
            t = self.last_w.get(k)
            if t is not None:
                deps.append(t)
        for k in writes:
            t = self.last_w.get(k)
            if t is not None:
                deps.append(t)
            deps.extend(self.readers.get(k, ()))
        return deps

    def _commit(self, tok, reads, writes):
        for k in reads:
            self.readers.setdefault(k, []).append(tok)
        for k in writes:
            self.last_w[k] = tok
            self.readers[k] = []

    def op(self, eng, fn, reads=(), writes=()):
        deps = self._deps(reads, writes)
        self.count[eng] += 1
        tok = ("c", eng, self.count[eng])
        self.ops[eng].append((fn, deps, tok))
        self._commit(tok, reads, writes)
        return tok

    def dma(self, eng, fn, reads=(), writes=(), semkey=None):
        deps = self._deps(reads, writes)
        if semkey is None:
            semkey = tuple(writes)
        if semkey not in self.dma_keys:
            pname = "sw" if eng == "pool" else "hw"
            pool = Sched.pools[pname]
            if pool:
                pool.sort(key=lambda x: -x[1])
                handle, cnt = pool.pop()
            else:
                Sched.nalloc += 1
                handle, cnt = self.nc.alloc_semaphore(name="d_%d" % Sched.nalloc), 0
            self.dma_keys[semkey] = [handle, cnt, eng, pname]
        ent = self.dma_keys[semkey]
        assert ent[2] == eng, "dma key used from two queues: %s" % (semkey,)
        ent[1] += 1
        tok = ("d", semkey, ent[1])
        self.ops[eng].append((fn, deps, tok))
        self._commit(tok, reads, writes)
        return tok

    def emit(self):
        nc = self.nc
        csem = Sched.csem
        dk = self.dma_keys
        finals = [(ent[0], ent[1]) for ent in dk.values()]
        cfinal = dict(self.count)
        with nc.Block() as block:

            def body(eng_name):
                def run(eng):
                    seen = {}
                    for fn, deps, tok in self.ops[eng_name]:
                        need = {}
                        for d in deps:
                            if d[0] == "c":
                                if d[1] == eng_name and (not self.same or eng_name == "pe"):
                                    continue
                                key = ("c", d[1])
                                val = d[2]
                            else:
                                key = ("d", d[1])
                                val = 16 * d[2]
                            if val > need.get(key, 0):
                                need[key] = val
                        for key, val in need.items():
                            if seen.get(key, 0) >= val:
                                continue
                            seen[key] = val
                            sem = csem[key[1]] if key[0] == "c" else dk[key[1]][0]
                            eng.wait_ge(sem, val)
                        ins = fn(eng)
                        if tok[0] == "c":
                            ins.then_inc(csem[eng_name], 1)
                        else:
                            ins.then_inc(dk[tok[1]][0], 16)
                    for handle, cnt in finals:
                        eng.wait_ge(handle, 16 * cnt)
                    for e2, cnt in cfinal.items():
                        if cnt > 0:
                            eng.wait_ge(csem[e2], cnt)
                return run

            block.tensor(body("pe"))
            block.scalar(body("act"))
            block.vector(body("dve"))
            block.gpsimd(body("pool"))
            block.sync(body("sp"))
        Sched.cbase = dict(self.count)
        for ent in dk.values():
            Sched.pools[ent[3]].append([ent[0], ent[1]])


class Ctx:
    pass


def declare_io(nc, debug, upto=99):
    D = Ctx()
    D.in_names = []

    def inp(name, shape, dt=F32):
        if upto < 5 and name in ("w_gate", "w_up", "w_down"):
            return None
        D.in_names.append(name)
        return nc.dram_tensor(name, list(shape), dt, kind="ExternalInput").ap()
    D.xk = inp("xk", [S_ALL, 1024])
    D.postok = inp("postok", [128, NT_ALL], I32)
    D.mem = inp("mem", [256, 1024])
    D.flagmask = inp("flagmask", [128, 128])
    D.ident = inp("ident", [128, 128])
    D.trimask = inp("trimask", [128, 128])
    D.norm_mix_g = inp("norm_mix_g", [1, 1024])
    D.w_in = inp("w_in", [1024, 1952])
    D.lam_q1 = inp("lam_q1", [1, 32]); D.lam_k1 = inp("lam_k1", [1, 32])
    D.lam_q2 = inp("lam_q2", [1, 32]); D.lam_k2 = inp("lam_k2", [1, 32])
    D.diff_out_g = inp("diff_out_g", [1, 64])
    D.mla_q_norm_g = inp("mla_q_norm_g", [1, 256])
    D.w_mla_uq = inp("w_mla_uq", [256, 768])
    D.mla_kv_norm_g = inp("mla_kv_norm_g", [1, 128])
    D.w_mla_ukv = inp("w_mla_ukv", [128, 1024])
    D.mla_out_g = inp("mla_out_g", [1, 512])
    D.w_out = inp("w_out", [1024, 1024])
    D.norm_cross_g = inp("norm_cross_g", [1, 1024])
    D.norm_mem_g = inp("norm_mem_g", [1, 1024])
    D.w_mem_q = inp("w_mem_q", [1024, 512])
    D.w_mem_kv = inp("w_mem_kv", [1024, 1024])
    D.w_mem_o = inp("w_mem_o", [512, 1024])
    D.norm_ffn_g = inp("norm_ffn_g", [1, 1024])
    D.w_grp = inp("w_grp", [1024, 4]); D.b_grp = inp("b_grp", [1, 4])
    D.w_exr = inp("w_exr", [1024, 32]); D.b_exr = inp("b_exr", [1, 32])
    D.w_gate = inp("w_gate", [32, 1024, 256])
    D.w_up = inp("w_up", [32, 1024, 256])
    D.w_down = inp("w_down", [32, 256, 1024])
    D.norm_final_g = inp("norm_final_g", [1, 1024])
    D.out = nc.dram_tensor("out", [S_OWN, 1024], F32, kind="ExternalOutput").ap()
    sk = "ExternalOutput" if debug else "Internal"
    scr = lambda name, shape, dt: nc.dram_tensor(name, list(shape), dt, kind=sk).ap()
    D.KdTc = scr("KdTc", [4, 128, S_ALL], BF16)
    D.QdTc = scr("QdTc", [4, 128, S_OWN], BF16)
    D.Vd = scr("Vd", [S_ALL, 520], BF16)
    D.KmT = scr("KmT", [8, 96, S_ALL], BF16)
    D.QmT = scr("QmT", [8, 96, S_OWN], BF16)
    D.Vm = scr("Vm", [S_ALL, 520], BF16)
    D.krow = scr("krow", [4, S_ALL], BF16)
    D.qrow = scr("qrow", [8, 4, S_OWN], BF16)
    D.AO = scr("AO", [S_OWN, 1024], BF16)
    D.SSM = scr("SSM", [128, NT_OWN], F32)
    D.X2 = scr("X2", [S_OWN, 1024], F32)
    D.H3T = scr("H3T", [128, 8, S_OWN], BF16)
    D.WRD = scr("WRD", [128, NT_OWN, 32], F32)
    return D


def rstd_ops(S, ss, rstd, n, key_ss, key_r):
    S.op("act", lambda e: e.activation(out=rstd, in_=ss, func=AF.Ln, scale=1.0 / n, bias=EPS),
         reads=[key_ss], writes=[key_r])
    S.op("act", lambda e: e.activation(out=rstd, in_=rstd, func=AF.Exp, scale=-0.5),
         reads=[key_r], writes=[key_r])


def phase1(nc, D):
    with contextlib.ExitStack() as st:
        T = lambda name, shape, dt: st.enter_context(nc.sbuf_tensor(name, list(shape), dt))
        PS = lambda name, shape, dt: st.enter_context(nc.psum_tensor(name, list(shape), dt))
        S = Sched(nc)
        idf = T("idf", [128, 128], F32)
        idb = T("idb", [128, 128], BF16)
        g_mix = T("g_mix", [128, 1024], F32)
        g_q = T("g_q", [128, 256], F32)
        g_kv = T("g_kv", [128, 128], F32)
        wb_in = T("wb_in", [128, 8, 1952], BF16)
        wb_uq = T("wb_uq", [128, 2, 768], BF16)
        wb_ukv = T("wb_ukv", [128, 1024], BF16)
        posi = T("posi", [128, NT_ALL], I32)
        posf = T("posf", [128, NT_ALL], F32)
        pa = T("pa", [128, NT_ALL], F32)
        pb = T("pb", [128, NT_ALL], F32)
        ang = T("ang", [128, NT_ALL, 16], F32)
        angm = T("angm", [128, NT_ALL, 16], F32)
        angi = T("angi", [128, NT_ALL, 16], I32)
        angf = T("angf", [128, NT_ALL, 16], F32)
        cosk = T("cosk", [128, NT_ALL, 16], F32)
        sink = T("sink", [128, NT_ALL, 16], F32)
        cosq = T("cosq", [128, NT_OWN, 16], F32)
        sinq = T("sinq", [128, NT_OWN, 16], F32)
        rowt = T("rowt", [128, 128], F32)
        cosq8 = T("cosq8", [128, NT_OWN, 8, 16], F32)
        sinq8 = T("sinq8", [128, NT_OWN, 8, 16], F32)
        qpe = T("qpe", [128, 8, 32], F32)
        rowb = T("rowb", [128, 128], BF16)
        xt = [T("xt%d" % i, [128, 1024], F32) for i in range(2)]
        junk = T("junk", [128, 1024], BF16)
        ssl = [T("ss%d" % i, [128, 1], F32) for i in range(3)]
        rsl = [T("rs%d" % i, [128, 1], F32) for i in range(3)]
        hn = T("hn", [128, 1024], BF16)
        hT = [T("hT%d" % i, [128, 8, 512], BF16) for i in range(2)]
        vd_t = [T("vd_t%d" % i, [128, 8, 65], BF16) for i in range(2)]
        vm_t = [T("vm_t%d" % i, [128, 8, 65], BF16) for i in range(2)]
        km_t = T("km_t", [128, 8, 96], BF16)
        qm_t = T("qm_t", [128, 8, 96], BF16)
        ckvn = T("ckvn", [128, 128], BF16)
        cqn = T("cqn", [128, 256], BF16)
        ckvnT = T("ckvnT", [128, 128], BF16)
        cqnT = T("cqnT", [128, 2, 128], BF16)
        kpe = T("kpe", [128, 32], F32)
        kr = T("kr", [128, 32], BF16)
        tmpa = T("tmpa", [128, 8, 16], F32)
        tmpb = T("tmpb", [128, 8, 16], F32)
        kmT = [T("kmT%d" % i, [96, 8, 512], BF16) for i in range(2)]
        qmT = [T("qmT%d" % i, [96, 8, 512], BF16) for i in range(2)]
        kdT = [T("kdT%d" % i, [128, 512], BF16) for i in range(4)]

        p_tr = PS("p_tr", [128, 8, 128], BF16)
        p_vd = PS("p_vd", [128, 512], F32)
        p_lat = PS("p_lat", [128, 512], F32)
        p_st = PS("p_st", [128, 8, 128], BF16)
        p_kv = PS("p_kv", [128, 1024], F32)
        p_mt = PS("p_mt", [128, 8, 128], BF16)
        p_fm = PS("p_fm", [128, 512], F32)

        S.dma("sp", lambda e: e.dma_start(out=idf[:], in_=D.ident[:, :]), writes=["idf"])
        S.op("dve", lambda e: e.tensor_copy(out=idb[:], in_=idf[:]), reads=["idf"], writes=["idb"])
        S.dma("sp", lambda e: e.dma_start(out=g_mix[:], in_=D.norm_mix_g.partition_broadcast(128)), writes=["g_mix"])
        S.dma("sp", lambda e: e.dma_start(out=g_q[:], in_=D.mla_q_norm_g.partition_broadcast(128)), writes=["g_q"])
        S.dma("sp", lambda e: e.dma_start(out=g_kv[:], in_=D.mla_kv_norm_g.partition_broadcast(128)), writes=["g_kv"])
        S.dma("sp", lambda e: e.dma_start(out=posi[:], in_=D.postok[:, :]), writes=["posi"])
        w_in_v = D.w_in.rearrange("(c p) n -> p c n", p=128)
        for c in range(8):
            S.dma("pool", lambda e, c=c: e.dma_start(out=wb_in[:, c, :], in_=w_in_v[:, c, :]), writes=["wb_in"])
        S.dma("pool", lambda e: e.dma_start(out=wb_uq[:], in_=D.w_mla_uq.rearrange("(c p) n -> p c n", p=128)),
              writes=["wb_uq"])
        S.dma("pool", lambda e: e.dma_start(out=wb_ukv[:], in_=D.w_mla_ukv[:, :]), writes=["wb_ukv"])

        STOP = int(os.environ.get("P1STOP", "99"))
        if STOP == 0:
            S.emit(); return
        S.op("dve", lambda e: e.tensor_copy(out=posf[:], in_=posi[:]), reads=["posi"], writes=["posf"])
        S.op("dve", lambda e: e.tensor_single_scalar(out=pa[:], in_=posf[:], scalar=1.0 / 64, op=ALU.mult),
             reads=["posf"], writes=["pa"])
        S.op("dve", lambda e: e.tensor_copy(out=posi[:], in_=pa[:]), reads=["pa", "posf"], writes=["posi"])
        S.op("dve", lambda e: e.tensor_copy(out=pb[:], in_=posi[:]), reads=["posi"], writes=["pb"])
        S.op("dve", lambda e: e.tensor_tensor(out=pa[:], in0=pa[:], in1=pb[:], op=ALU.subtract),
             reads=["pa", "pb"], writes=["pa"])
        S.op("dve", lambda e: e.tensor_single_scalar(out=pa[:], in_=pa[:], scalar=0.0, op=ALU.is_lt),
             reads=["pa"], writes=["pa"])
        S.op("dve", lambda e: e.tensor_tensor(out=pa[:], in0=pb[:], in1=pa[:], op=ALU.subtract),
             reads=["pa", "pb"], writes=["pa"])
        S.op("dve", lambda e: e.scalar_tensor_tensor(out=pb[:], in0=pa[:], scalar=-64.0, in1=posf[:],
                                                     op0=ALU.mult, op1=ALU.add),
             reads=["pa", "posf"], writes=["pb"])
        for j in range(16):
            S.op("dve", lambda e, j=j: e.tensor_single_scalar(out=ang[:, :, j], in_=posf[:], scalar=INV_FREQ[j],
                                                              op=ALU.mult), reads=["posf"], writes=["ang"])
        S.op("dve", lambda e: e.tensor_single_scalar(out=ang[:], in_=ang[:], scalar=1.0 / TWO_PI, op=ALU.mult),
             reads=["ang"], writes=["ang"])

        def sin_turns(dst, key, shift):
            S.op("dve", lambda e: e.tensor_single_scalar(out=angm[:], in_=ang[:], scalar=shift, op=ALU.add),
                 reads=["ang"], writes=["angm"])
            S.op("dve", lambda e: e.tensor_copy(out=angi[:], in_=angm[:]), reads=["angm"], writes=["angi"])
            S.op("dve", lambda e: e.tensor_copy(out=angf[:], in_=angi[:]), reads=["angi"], writes=["angf"])
            S.op("dve", lambda e: e.tensor_tensor(out=angm[:], in0=angm[:], in1=angf[:], op=ALU.subtract),
                 reads=["angm", "angf"], writes=["angm"])
            S.op("dve", lambda e: e.tensor_single_scalar(out=angf[:], in_=angm[:], scalar=0.5, op=ALU.is_ge),
                 reads=["angm"], writes=["angf"])
            S.op("dve", lambda e: e.tensor_tensor(out=angm[:], in0=angm[:], in1=angf[:], op=ALU.subtract),
                 reads=["angm", "angf"], writes=["angm"])
            S.op("act", lambda e: e.activation(out=dst, in_=angm[:], func=AF.Sin, scale=TWO_PI),
                 reads=["angm"], writes=[key])

        sin_turns(sink[:], "sink", 0.0)
        sin_turns(cosk[:], "cosk", 0.25)
        S.op("dve", lambda e: e.tensor_single_scalar(out=cosq[:], in_=cosk[:, 0:NT_OWN, :], scalar=SC_M, op=ALU.mult),
             reads=["cosk"], writes=["cosq"])
        S.op("dve", lambda e: e.tensor_single_scalar(out=sinq[:], in_=sink[:, 0:NT_OWN, :], scalar=SC_M, op=ALU.mult),
             reads=["sink"], writes=["sinq"])

        for h in range(8):
            S.op("pool", lambda e, h=h: e.tensor_copy(out=cosq8[:, :, h, :], in_=cosq[:]), reads=["cosq"], writes=["cosq8"])
            S.op("pool", lambda e, h=h: e.tensor_copy(out=sinq8[:, :, h, :], in_=sinq[:]), reads=["sinq"], writes=["sinq8"])
        def row_flush(dst_ap, npart):
            S.op("pe", lambda e: e.transpose(out=p_fm[:, 0:128], in_=rowt[:], identity=idf[:]),
                 reads=["rowt", "idf"], writes=["p_fm"])
            S.op("dve", lambda e: e.tensor_copy(out=rowb[:], in_=p_fm[:, 0:128]), reads=["p_fm"], writes=["rowb"])
            S.dma("sp", lambda e: e.dma_start(out=dst_ap, in_=rowb[0:npart, :]), reads=["rowb"], writes=["rows_dram"])

        S.op("dve", lambda e: e.tensor_copy(out=rowt[:, 0:64], in_=pa[:]), reads=["pa"], writes=["rowt"])
        S.op("dve", lambda e: e.tensor_copy(out=rowt[:, 64:128], in_=pb[:]), reads=["pb"], writes=["rowt"])
        row_flush(D.krow[0:2, :].rearrange("r (t p) -> (r t) p", p=128), 128)
        S.op("dve", lambda e: e.memset(rowt[:], 1.0), reads=["p_fm"], writes=["rowt"])
        row_flush(D.krow[2:4, :].rearrange("r (t p) -> (r t) p", p=128), 128)
        for h in range(8):
            s = SLOPES[h]
            S.op("dve", lambda e, s=s: e.memset(rowt[:, 0:32], 64.0 * s), reads=["p_fm"], writes=["rowt"])
            S.op("dve", lambda e, s=s: e.memset(rowt[:, 32:64], s), writes=["rowt"])
            S.op("dve", lambda e, s=s: e.tensor_single_scalar(out=rowt[:, 64:96], in_=pa[:, 0:NT_OWN], scalar=-64.0 * s,
                                                              op=ALU.mult), reads=["pa"], writes=["rowt"])
            S.op("dve", lambda e, s=s: e.tensor_single_scalar(out=rowt[:, 96:128], in_=pb[:, 0:NT_OWN], scalar=-s,
                                                              op=ALU.mult), reads=["pb"], writes=["rowt"])
            row_flush(D.qrow[h].rearrange("r (t p) -> (r t) p", p=128), 128)
        if STOP == 1:
            S.emit(); return
        if STOP == 2:
            S.emit(); return
        for i in range(2):
            S.op("pool", lambda e, i=i: e.memset(vd_t[i][:], 1.0), writes=["vd_t%d" % i])
            S.op("pool", lambda e, i=i: e.memset(vm_t[i][:], 1.0), writes=["vm_t%d" % i])

        NTL = int(os.environ.get("P1NT", str(NT_ALL)))
        CUT = int(os.environ.get("P1CUT", "99"))
        hn2 = [hn, T("hn_b", [128, 1024], BF16)]
        ssx = [T("ssx%d" % i, [128, 1], F32) for i in range(2)]
        rsx = [T("rsx%d" % i, [128, 1], F32) for i in range(2)]

        def norm_x(tt):
            xb_ = tt % 2
            S.op("act", lambda e: e.activation(out=junk[:], in_=xt[xb_][:], func=AF.Square, accum_out=ssx[xb_][:]),
                 reads=["xt%d" % xb_], writes=["junk", "ssx%d" % xb_])
            rstd_ops(S, ssx[xb_][:], rsx[xb_][:], 1024, "ssx%d" % xb_, "rsx%d" % xb_)
            S.op("dve", lambda e: e.scalar_tensor_tensor(out=hn2[xb_][:], in0=xt[xb_][:], scalar=rsx[xb_][:], in1=g_mix[:],
                                                         op0=ALU.mult, op1=ALU.mult),
                 reads=["xt%d" % xb_, "rsx%d" % xb_, "g_mix"], writes=["hn%d" % xb_])

        for t in range(NTL):
            own = t < NT_OWN
            s_i = t // 4
            j = t % 4
            hs = s_i % 2
            xb = t % 2
            tok0 = t * 128
            if t == 0:
                S.dma("sp", lambda e: e.dma_start(out=xt[0][:], in_=D.xk[0:128, :]), writes=["xt0"])
            if t + 1 < NTL:
                S.dma("sp", lambda e, t=t: e.dma_start(out=xt[(t + 1) % 2][:], in_=D.xk[(t + 1) * 128:(t + 2) * 128, :]),
                      writes=["xt%d" % ((t + 1) % 2)])
            if t == 0:
                norm_x(0)
            for c in range(8):
                S.op("pe", lambda e, c=c, xb=xb: e.transpose(out=p_tr[:, c, :], in_=hn2[xb][:, c * 128:(c + 1) * 128],
                                                            identity=idb[:]),
                     reads=["hn%d" % xb, "idb"], writes=["p_tr"])
            hT_key = "hT%d" % hs
            S.op("dve", lambda e, hs=hs, j=j: e.tensor_copy(out=hT[hs][:, :, j * 128:(j + 1) * 128], in_=p_tr[:]),
                 reads=["p_tr"], writes=[hT_key])
            if t + 1 < NTL:
                norm_x(t + 1)
            lhs = lambda c, hs=hs, j=j: hT[hs][:, c, j * 128:(j + 1) * 128]
            if CUT == 1:
                break
            for c in range(8):
                S.op("pe", lambda e, c=c, lhs=lhs: e.matmul(p_vd[:], lhsT=lhs(c), rhs=wb_in[:, c, 1024:1536],
                                                            start=(c == 0), stop=(c == 7)),
                     reads=[hT_key, "wb_in"], writes=["p_vd"])
            vb = t % 2
            S.op("act", lambda e, vb=vb: e.activation(out=vd_t[vb][:, :, 0:64],
                                                      in_=p_vd[:].rearrange("p (h d) -> p h d", d=64), func=AF.Copy),
                 reads=["p_vd"], writes=["vd_t%d" % vb])
            S.dma("sp", lambda e, vb=vb, tok0=tok0: e.dma_start(
                out=D.Vd[tok0:tok0 + 128, :], in_=vd_t[vb][:].rearrange("p h d -> p (h d)")),
                reads=["vd_t%d" % vb], writes=["Vd"])
            if CUT == 2:
                break
            lo = 1536 if own else 1792
            nlat = 1952 - lo
            for c in range(8):
                S.op("pe", lambda e, c=c, lhs=lhs, lo=lo, nlat=nlat: e.matmul(
                    p_lat[:, 0:nlat], lhsT=lhs(c), rhs=wb_in[:, c, lo:1952], start=(c == 0), stop=(c == 7)),
                    reads=[hT_key, "wb_in"], writes=["p_lat"])
            okv = 256 if own else 0
            if CUT == 3:
                break
            S.op("act", lambda e, okv=okv: e.activation(out=junk[:, 0:128], in_=p_lat[:, okv:okv + 128], func=AF.Square,
                                                        accum_out=ssl[1][:]),
                 reads=["p_lat"], writes=["junk", "ss1"])
            rstd_ops(S, ssl[1][:], rsl[1][:], 128, "ss1", "rs1")
            if CUT == 31:
                break
            S.op("dve", lambda e, okv=okv: e.scalar_tensor_tensor(out=ckvn[:], in0=p_lat[:, okv:okv + 128], scalar=rsl[1][:],
                                                                  in1=g_kv[:], op0=ALU.mult, op1=ALU.mult),
                 reads=["p_lat", "rs1", "g_kv"], writes=["ckvn"])
            if CUT == 32:
                break
            S.op("pe", lambda e: e.transpose(out=p_st[:, 0, :], in_=ckvn[:], identity=idb[:]),
                 reads=["ckvn", "idb"], writes=["p_st"])
            S.op("dve", lambda e: e.tensor_copy(out=ckvnT[:], in_=p_st[:, 0, :]), reads=["p_st"], writes=["ckvnT"])
            if CUT == 33:
                break
            for half in range(2):
                S.op("pe", lambda e, half=half: e.matmul(p_kv[:, half * 512:(half + 1) * 512], lhsT=ckvnT[:],
                                                         rhs=wb_ukv[:, half * 512:(half + 1) * 512], start=True, stop=True),
                     reads=["ckvnT", "wb_ukv"], writes=["p_kv"])
            if CUT == 34:
                break
            kvv = p_kv[:].rearrange("p (h x) -> p h x", x=128)
            S.op("act", lambda e, vb=vb, kvv=kvv: e.activation(out=vm_t[vb][:, :, 0:64], in_=kvv[:, :, 64:128], func=AF.Copy),
                 reads=["p_kv"], writes=["vm_t%d" % vb])
            if CUT == 35:
                break
            S.dma("sp", lambda e, vb=vb, tok0=tok0: e.dma_start(
                out=D.Vm[tok0:tok0 + 128, :], in_=vm_t[vb][:].rearrange("p h d -> p (h d)")),
                reads=["vm_t%d" % vb], writes=["Vm"])
            S.op("act", lambda e, kvv=kvv: e.activation(out=km_t[:, :, 0:64], in_=kvv[:, :, 0:64], func=AF.Copy),
                 reads=["p_kv"], writes=["km_t"])
            if CUT == 4:
                break
            S.op("act", lambda e, okv=okv: e.activation(out=kpe[:], in_=p_lat[:, okv + 128:okv + 160], func=AF.Copy),
                 reads=["p_lat"], writes=["kpe"])
            ta = tmpa[:, 0, :]
            tb = tmpb[:, 0, :]
            ck = cosk[:, t, :]
            sk_ = sink[:, t, :]
            S.op("dve", lambda e, ck=ck, ta=ta: e.tensor_tensor(out=ta, in0=kpe[:, 0:16], in1=ck, op=ALU.mult),
                 reads=["kpe", "cosk"], writes=["tmpa"])
            S.op("dve", lambda e, sk_=sk_, tb=tb: e.tensor_tensor(out=tb, in0=kpe[:, 16:32], in1=sk_, op=ALU.mult),
                 reads=["kpe", "sink"], writes=["tmpb"])
            S.op("dve", lambda e, ta=ta, tb=tb: e.tensor_tensor(out=kr[:, 0:16], in0=ta, in1=tb, op=ALU.subtract),
                 reads=["tmpa", "tmpb"], writes=["kr"])
            S.op("dve", lambda e, ck=ck, ta=ta: e.tensor_tensor(out=ta, in0=kpe[:, 16:32], in1=ck, op=ALU.mult),
                 reads=["kpe", "cosk", "kr"], writes=["tmpa"])
            S.op("dve", lambda e, sk_=sk_, tb=tb: e.tensor_tensor(out=tb, in0=kpe[:, 0:16], in1=sk_, op=ALU.mult),
                 reads=["kpe", "sink", "kr"], writes=["tmpb"])
            S.op("dve", lambda e, ta=ta, tb=tb: e.tensor_tensor(out=kr[:, 16:32], in0=ta, in1=tb, op=ALU.add),
                 reads=["tmpa", "tmpb"], writes=["kr"])
            if CUT == 5:
                break
            for h in range(8):
                if h % 2 == 0:
                    S.op("act", lambda e, h=h: e.activation(out=km_t[:, h, 64:96], in_=kr[:], func=AF.Copy),
                         reads=["kr"], writes=["km_pe%d" % h])
                else:
                    S.op("dve", lambda e, h=h: e.tensor_copy(out=km_t[:, h, 64:96], in_=kr[:]),
                         reads=["kr"], writes=["km_pe%d" % h])
            for h in range(8):
                S.op("pe", lambda e, h=h: e.transpose(out=p_mt[0:96, h, :], in_=km_t[:, h, :], identity=idb[:]),
                     reads=["km_t", "km_pe%d" % h, "idb"], writes=["p_mt"])
            S.op("dve", lambda e, hs=hs, j=j: e.tensor_copy(out=kmT[hs][:, :, j * 128:(j + 1) * 128], in_=p_mt[0:96, :, :]),
                 reads=["p_mt"], writes=["kmT%d" % hs])
            if CUT == 6:
                break
            if own:
                S.op("act", lambda e: e.activation(out=junk[:, 0:256], in_=p_lat[:, 0:256], func=AF.Square,
                                                   accum_out=ssl[2][:]), reads=["p_lat"], writes=["junk", "ss2"])
                rstd_ops(S, ssl[2][:], rsl[2][:], 256, "ss2", "rs2")
                S.op("dve", lambda e: e.scalar_tensor_tensor(out=cqn[:], in0=p_lat[:, 0:256], scalar=rsl[2][:], in1=g_q[:],
                                                             op0=ALU.mult, op1=ALU.mult),
                     reads=["p_lat", "rs2", "g_q"], writes=["cqn"])
                for c in range(2):
                    S.op("pe", lambda e, c=c: e.transpose(out=p_st[:, 1 + c, :], in_=cqn[:, c * 128:(c + 1) * 128],
                                                          identity=idb[:]), reads=["cqn", "idb"], writes=["p_st"])
                S.op("dve", lambda e: e.tensor_copy(out=cqnT[:], in_=p_st[:, 1:3, :]), reads=["p_st"], writes=["cqnT"])
                for (c0, c1) in ((0, 512), (512, 768)):
                    for c in range(2):
                        S.op("pe", lambda e, c=c, c0=c0, c1=c1: e.matmul(p_kv[:, c0:c1], lhsT=cqnT[:, c, :],
                                                                         rhs=wb_uq[:, c, c0:c1], start=(c == 0), stop=(c == 1)),
                             reads=["cqnT", "wb_uq"], writes=["p_kv"])
                qv = p_kv[:, 0:768].rearrange("p (h x) -> p h x", x=96)
                S.op("act", lambda e, qv=qv: e.activation(out=qm_t[:, :, 0:64], in_=qv[:, :, 0:64], func=AF.Copy, scale=SC_M),
                     reads=["p_kv"], writes=["qm_t"])
                S.op("act", lambda e, qv=qv: e.activation(out=qpe[:], in_=qv[:, :, 64:96], func=AF.Copy),
                     reads=["p_kv"], writes=["qpe"])
                cq = cosq8[:, t, :, :]
                sq = sinq8[:, t, :, :]
                x1 = qpe[:, :, 0:16]
                x2 = qpe[:, :, 16:32]
                S.op("dve", lambda e, x1=x1, cq=cq: e.tensor_tensor(out=tmpa[:], in0=x1, in1=cq, op=ALU.mult),
                     reads=["qpe", "cosq8"], writes=["tmpa"])
                S.op("dve", lambda e, x2=x2, sq=sq: e.tensor_tensor(out=tmpb[:], in0=x2, in1=sq, op=ALU.mult),
                     reads=["qpe", "sinq8"], writes=["tmpb"])
                S.op("dve", lambda e: e.tensor_tensor(out=qm_t[:, :, 64:80], in0=tmpa[:], in1=tmpb[:], op=ALU.subtract),
                     reads=["tmpa", "tmpb"], writes=["qm_t"])
                S.op("dve", lambda e, x2=x2, cq=cq: e.tensor_tensor(out=tmpa[:], in0=x2, in1=cq, op=ALU.mult),
                     reads=["qpe", "cosq8", "qm_t"], writes=["tmpa"])
                S.op("dve", lambda e, x1=x1, sq=sq: e.tensor_tensor(out=tmpb[:], in0=x1, in1=sq, op=ALU.mult),
                     reads=["qpe", "sinq8", "qm_t"], writes=["tmpb"])
                S.op("dve", lambda e: e.tensor_tensor(out=qm_t[:, :, 80:96], in0=tmpa[:], in1=tmpb[:], op=ALU.add),
                     reads=["tmpa", "tmpb"], writes=["qm_t"])
                for h in range(8):
                    S.op("pe", lambda e, h=h: e.transpose(out=p_mt[0:96, h, :], in_=qm_t[:, h, :], identity=idb[:]),
                         reads=["qm_t", "idb"], writes=["p_mt"])
                S.op("dve", lambda e, hs=hs, j=j: e.tensor_copy(out=qmT[hs][:, :, j * 128:(j + 1) * 128], in_=p_mt[0:96, :, :]),
                     reads=["p_mt"], writes=["qmT%d" % hs])
            if j == 3:
                st0 = s_i * 512
                S.dma("sp", lambda e, hs=hs, st0=st0: e.dma_start(
                    out=D.KmT[:, :, st0:st0 + 512].rearrange("h r t -> r h t"), in_=kmT[hs][:]),
                    reads=["kmT%d" % hs], writes=["KmT"])
                if own:
                    S.dma("sp", lambda e, hs=hs, st0=st0: e.dma_start(
                        out=D.QmT[:, :, st0:st0 + 512].rearrange("h r t -> r h t"), in_=qmT[hs][:]),
                        reads=["qmT%d" % hs], writes=["QmT"])
                jobs = [("k", cc) for cc in range(4)] + ([("q", cc) for cc in range(4)] if own else [])
                for ji, (kind, cc) in enumerate(jobs):
                    col0 = (512 if kind == "k" else 0) + cc * 128
                    pbank, pkey = (p_fm, "p_fm") if ji % 2 == 0 else (p_vd, "p_vd")
                    for c in range(8):
                        S.op("pe", lambda e, c=c, col0=col0, hs=hs, pbank=pbank: e.matmul(
                            pbank[:], lhsT=wb_in[:, c, col0:col0 + 128], rhs=hT[hs][:, c, :], start=(c == 0), stop=(c == 7)),
                            reads=[hT_key, "wb_in"], writes=[pkey])
                    fb = ji % 4
                    sc = 1.0 if kind == "k" else SC_D
                    S.op("act", lambda e, fb=fb, pbank=pbank, sc=sc: e.activation(out=kdT[fb][:], in_=pbank[:], func=AF.Copy, scale=sc),
                         reads=[pkey], writes=["kdT%d" % fb])
                    dst = D.KdTc if kind == "k" else D.QdTc
                    S.dma("act", lambda e, fb=fb, cc=cc, dst=dst, st0=st0: e.dma_start(
                        out=dst[cc, :, st0:st0 + 512], in_=kdT[fb][:]),
                        reads=["kdT%d" % fb], writes=["KQdT"])
        S.emit()


def attn_phase(nc, D, kind):
    R = 36 if kind == "d" else 96
    NM = 2 if kind == "d" else 1
    Vsrc = D.Vd if kind == "d" else D.Vm
    NH = int(os.environ.get("ATT_NH", "8"))
    NI = int(os.environ.get("ATT_NI", "8"))
    with contextlib.ExitStack() as st:
        T = lambda name, shape, dt: st.enter_context(nc.sbuf_tensor(name, list(shape), dt))
        PS = lambda name, shape, dt: st.enter_context(nc.psum_tensor(name, list(shape), dt))
        S = Sched(nc)
        idf = T(kind + "_idf", [128, 128], F32)
        idb = T(kind + "_idb", [128, 128], BF16)
        mtmp = T(kind + "_mtmp", [128, 128], F32)
        trib = T(kind + "_trib", [128, 128], BF16)
        flagb = T(kind + "_flagb", [128, 128], BF16)
        V = T(kind + "_V", [128, NT_ALL, 520], BF16)
        if kind == "d":
            KTd = [T(kind + "_KT%d" % b, [128, S_ALL], BF16) for b in range(2)]
            QTd = [T(kind + "_QT%d" % b, [128, S_OWN], BF16) for b in range(2)]
            KT = [[KTd[b][0:36, :], KTd[b][64:100, :]] for b in range(2)]
            QT = [[QTd[b][0:36, :], QTd[b][64:100, :]] for b in range(2)]
        else:
            KT = [[T(kind + "_KT%d%d" % (b, m), [R, S_ALL], BF16)[:, :] for m in range(NM)] for b in range(2)]
            QT = [[T(kind + "_QT%d%d" % (b, m), [R, S_OWN], BF16)[:, :] for m in range(NM)] for b in range(2)]
        pT = [T(kind + "_pT%d" % i, [128, 2, 512], BF16) for i in range(3)]
        accs = T(kind + "_accs", [65, NM, 512], F32)
        o1 = T(kind + "_o1", [128, 64], F32)
        o2 = T(kind + "_o2", [128, 64], F32)
        ob = [T(kind + "_ob%d" % i, [128, 64], BF16) for i in range(2)]
        junk = T(kind + "_junk", [128, 64], BF16)
        r1 = T(kind + "_r1", [128, 1], F32)
        r2 = T(kind + "_r2", [128, 1], F32)
        lam2 = T(kind + "_lam2", [128, 1], F32)
        ss = T(kind + "_ss", [128, 1], F32)
        rs = T(kind + "_rs", [128, 1], F32)
        neglam = T(kind + "_neglam", [128, 1], F32)
        ssacc = T(kind + "_ssacc", [128, NT_OWN], F32)
        lv = [T(kind + "_lv%d" % i, [128, 32], F32) for i in range(4)]
        e1 = T(kind + "_e1", [128, 1], F32)
        e2 = T(kind + "_e2", [128, 1], F32)
        sT = [PS(kind + "_sT%d" % i, [128, 2, 512], F32) for i in range(2)]
        acc = [PS(kind + "_acc%d" % i, [128, 512], F32) for i in range(2)]
        ptr = PS(kind + "_ptr", [128, 2, 128], F32)
        pwarm = PS(kind + "_pwarm", [128, 512], F32)
        WARM = int(os.environ.get("ATT_WARM", "1"))

        S.dma("sp", lambda e: e.dma_start(out=idf[:], in_=D.ident[:, :]), writes=["idf"])
        S.op("dve", lambda e: e.tensor_copy(out=idb[:], in_=idf[:]), reads=["idf"], writes=["idb"])
        S.dma("sp", lambda e: e.dma_start(out=mtmp[:], in_=D.trimask[:, :]), writes=["mtmp"])
        S.op("dve", lambda e: e.tensor_copy(out=trib[:], in_=mtmp[:]), reads=["mtmp"], writes=["trib"])
        S.dma("sp", lambda e: e.dma_start(out=mtmp[:], in_=D.flagmask[:, :]), reads=["trib"], writes=["mtmp"])
        S.op("dve", lambda e: e.tensor_copy(out=flagb[:], in_=mtmp[:]), reads=["mtmp"], writes=["flagb"])
        Vv = Vsrc.rearrange("(t p) c -> p t c", p=128)
        for c in range(8):
            S.dma("sp", lambda e, c=c: e.dma_start(out=V[:, c * 8:(c + 1) * 8, :], in_=Vv[:, c * 8:(c + 1) * 8, :]),
                  writes=["V"])
        if kind == "d":
            srcs = [D.lam_q1, D.lam_k1, D.lam_q2, D.lam_k2]
            for i in range(4):
                S.dma("sp", lambda e, i=i: e.dma_start(out=lv[i][:], in_=srcs[i].partition_broadcast(128)), writes=["lv%d" % i])
            S.op("dve", lambda e: e.tensor_tensor(out=lv[0][:], in0=lv[0][:], in1=lv[1][:], op=ALU.mult),
                 reads=["lv0", "lv1"], writes=["lv0"])
            S.op("dve", lambda e: e.tensor_tensor(out=lv[2][:], in0=lv[2][:], in1=lv[3][:], op=ALU.mult),
                 reads=["lv2", "lv3"], writes=["lv2"])
            S.op("act", lambda e: e.activation(out=lv[1][:], in_=lv[0][:], func=AF.Copy, accum_out=e1[:]),
                 reads=["lv0", "lv1"], writes=["lv1", "e1"])
            S.op("act", lambda e: e.activation(out=lv[3][:], in_=lv[2][:], func=AF.Copy, accum_out=e2[:]),
                 reads=["lv2", "lv3"], writes=["lv3", "e2"])
            S.op("act", lambda e: e.activation(out=e1[:], in_=e1[:], func=AF.Exp), reads=["e1"], writes=["e1"])
            S.op("act", lambda e: e.activation(out=e2[:], in_=e2[:], func=AF.Exp), reads=["e2"], writes=["e2"])
            S.op("dve", lambda e: e.tensor_tensor(out=neglam[:], in0=e2[:], in1=e1[:], op=ALU.subtract),
                 reads=["e1", "e2"], writes=["neglam"])
            S.op("dve", lambda e: e.tensor_single_scalar(out=neglam[:], in_=neglam[:], scalar=-0.2, op=ALU.add),
                 reads=["neglam"], writes=["neglam"])
        else:
            S.op("dve", lambda e: e.memset(ssacc[:], 0.0), writes=["ssacc"])

        if kind == "d":
            for b in range(2):
                S.op("pool", lambda e, b=b: e.memset(KTd[b][:], 0.0), writes=["KT%d0" % b, "KT%d1" % b])
                S.op("pool", lambda e, b=b: e.memset(QTd[b][:], 0.0), writes=["QT%d0" % b, "QT%d1" % b])
        gcount = 0
        acount = 0
        ocnt = [0]
        deferred = []
        for h in range(NH):
            b = h % 2
            for m in range(NM):
                if kind == "d":
                    cc = h // 2
                    g0 = ((h % 2) * 2 + m) * 32
                    for c in range(4):
                        cs = slice(c * 2048, (c + 1) * 2048)
                        S.dma("sp", lambda e, b=b, m=m, cs=cs, cc=cc, g0=g0: e.dma_start(
                            out=KT[b][m][0:32, cs], in_=D.KdTc[cc, g0:g0 + 32, cs]), writes=["KT%d%d" % (b, m)])
                    S.dma("sp", lambda e, b=b, m=m: e.dma_start(out=KT[b][m][32:36, :], in_=D.krow[:, :]),
                          writes=["KT%d%d" % (b, m)])
                    S.dma("sp", lambda e, b=b, m=m, cc=cc, g0=g0: e.dma_start(
                        out=QT[b][m][0:32, :], in_=D.QdTc[cc, g0:g0 + 32, :]), writes=["QT%d%d" % (b, m)])
                    S.dma("sp", lambda e, b=b, m=m, h=h: e.dma_start(out=QT[b][m][32:36, :], in_=D.qrow[h]),
                          writes=["QT%d%d" % (b, m)])
                    continue
                ksrc = D.KmT[h]
                qsrc = D.QmT[h]
                for c in range(4):
                    S.dma("sp", lambda e, b=b, m=m, c=c, ksrc=ksrc: e.dma_start(
                        out=KT[b][m][:, c * 2048:(c + 1) * 2048], in_=ksrc[:, c * 2048:(c + 1) * 2048]),
                        writes=["KT%d%d" % (b, m)])
                S.dma("sp", lambda e, b=b, m=m, qsrc=qsrc: e.dma_start(out=QT[b][m], in_=qsrc[:, :]),
                      writes=["QT%d%d" % (b, m)])
            for I in range(NI):
                if kind == "d":
                    nk = 4 * I + 4
                    pending = None

                    def emit_av2(p, h=h):
                        ktile, c0, pb, first, last = p
                        for m in range(2):
                            S.op("pe", lambda e, m=m: e.matmul(
                                acc[m][0:65, c0:512], lhsT=V[:, ktile, h * 65:(h + 1) * 65], rhs=pT[pb][:, m, c0:512],
                                start=first, stop=last),
                                reads=["V", "pT%d" % pb], writes=["acc%d" % m])
                        if WARM:
                            S.op("pe", lambda e: e.matmul(pwarm[:, c0:min(512, c0 + 384)], lhsT=V[:, ktile, 0:128], rhs=pT[pb][:, 0, c0:min(512, c0 + 384)],
                                                          start=True, stop=True),
                                 reads=["V", "pT%d" % pb], writes=["pwarm"])

                    for kt in range(nk):
                        a = kt - 4 * I
                        c0 = max(0, a) * 128
                        for g in range(2):
                            sb = gcount % 2
                            pb = gcount % 3
                            gcount += 1
                            koff = kt * 128 if g == 0 else S_OWN + kt * 128
                            ktile = kt if g == 0 else NT_OWN + kt
                            mask = None if a < 0 else (trib if g == 0 else flagb)
                            for m in range(2):
                                S.op("pe", lambda e, m=m, koff=koff, c0=c0, sb=sb, mask=mask, b=b, I=I: e.matmul(
                                    sT[sb][:, m, c0:512], lhsT=KT[b][m][:, koff:koff + 128],
                                    rhs=QT[b][m][:, I * 512 + c0:I * 512 + 512], start=True, stop=(mask is None)),
                                    reads=["KT%d%d" % (b, m), "QT%d%d" % (b, m)], writes=["sT%d" % sb])
                            if mask is not None:
                                for m in range(2):
                                    S.op("pe", lambda e, m=m, c0=c0, sb=sb, mask=mask: e.matmul(
                                        sT[sb][:, m, c0:c0 + 128], lhsT=idb[:], rhs=mask[:], start=False, stop=True),
                                        reads=["idb", "trib", "flagb"], writes=["sT%d" % sb])
                            if pending is not None:
                                emit_av2(pending)
                            S.op("act", lambda e, sb=sb, pb=pb, c0=c0: e.activation(
                                out=pT[pb][:, :, c0:512], in_=sT[sb][:, :, c0:512], func=AF.Exp),
                                reads=["sT%d" % sb], writes=["pT%d" % pb])
                            pending = (ktile, c0, pb, kt == 0 and g == 0, kt == nk - 1 and g == 1)
                            if deferred:
                                deferred.pop(0)()
                    emit_av2(pending)
                    while deferred:
                        deferred.pop(0)()
                    for m in range(2):
                        S.op("dve", lambda e, m=m: e.tensor_copy(out=accs[:, m, :], in_=acc[m][0:65, :]),
                             reads=["acc%d" % m], writes=["accs%d" % m])
                else:
                    for m in range(NM):
                        ab = acount % 2
                        acount += 1
                        nk = 4 * I + 4
                        pending = None

                        def emit_av(p, m=m, h=h):
                            kt, c0, pb, ab_, first, last = p
                            for g in range(2):
                                ktile = kt if g == 0 else NT_OWN + kt
                                S.op("pe", lambda e, g=g, ktile=ktile, c0=c0, pb=pb, ab_=ab_, first=first, last=last: e.matmul(
                                    acc[ab_][0:65, c0:512], lhsT=V[:, ktile, h * 65:(h + 1) * 65], rhs=pT[pb][:, g, c0:512],
                                    start=(first and g == 0), stop=(last and g == 1)),
                                    reads=["V", "pT%d" % pb], writes=["acc%d" % ab_])

                        for kt in range(nk):
                            a = kt - 4 * I
                            c0 = max(0, a) * 128
                            sb = gcount % 2
                            pb = gcount % 3
                            gcount += 1
                            for g in range(2):
                                koff = kt * 128 if g == 0 else S_OWN + kt * 128
                                mask = None if a < 0 else (trib if g == 0 else flagb)
                                S.op("pe", lambda e, g=g, koff=koff, c0=c0, sb=sb, b=b, m=m, I=I, mask=mask: e.matmul(
                                    sT[sb][:, g, c0:512], lhsT=KT[b][m][:, koff:koff + 128],
                                    rhs=QT[b][m][:, I * 512 + c0:I * 512 + 512], start=True, stop=(mask is None)),
                                    reads=["KT%d%d" % (b, m), "QT%d%d" % (b, m)], writes=["sT%d" % sb])
                                if mask is not None:
                                    S.op("pe", lambda e, g=g, c0=c0, sb=sb, mask=mask: e.matmul(
                                        sT[sb][:, g, c0:c0 + 128], lhsT=idb[:], rhs=mask[:], start=False, stop=True),
                                        reads=["idb", "trib", "flagb"], writes=["sT%d" % sb])
                            if pending is not None:
                                emit_av(pending)
                            S.op("act", lambda e, sb=sb, pb=pb, c0=c0: e.activation(
                                out=pT[pb][:, :, c0:512], in_=sT[sb][:, :, c0:512], func=AF.Exp),
                                reads=["sT%d" % sb], writes=["pT%d" % pb])
                            pending = (kt, c0, pb, ab, kt == 0, kt == nk - 1)
                            if deferred:
                                deferred.pop(0)()
                        emit_av(pending)
                        while deferred:
                            deferred.pop(0)()
                        S.op("dve", lambda e, m=m, ab=ab: e.tensor_copy(out=accs[:, m, :], in_=acc[ab][0:65, :]),
                             reads=["acc%d" % ab], writes=["accs%d" % m])
                def epi(qt, I=I, h=h):
                    tile_i = I * 4 + qt
                    tok0 = tile_i * 128
                    for m in range(NM):
                        S.op("pe", lambda e, m=m, qt=qt: e.transpose(out=ptr[:, m, 0:65], in_=accs[:, m, qt * 128:(qt + 1) * 128],
                                                                    identity=idf[0:65, 0:65]),
                             reads=["accs%d" % m, "idf"], writes=["ptr"])
                    obi = ocnt[0] % 2
                    ocnt[0] += 1
                    S.op("dve", lambda e: e.reciprocal(out=r1[:], in_=ptr[:, 0, 64:65]), reads=["ptr"], writes=["r1"])
                    S.op("dve", lambda e: e.tensor_scalar(out=o1[:], in0=ptr[:, 0, 0:64], scalar1=r1[:], scalar2=None,
                                                          op0=ALU.mult), reads=["ptr", "r1"], writes=["o1"])
                    if kind == "d":
                        S.op("dve", lambda e: e.reciprocal(out=r2[:], in_=ptr[:, 1, 64:65]), reads=["ptr"], writes=["r2"])
                        S.op("dve", lambda e: e.tensor_tensor(out=lam2[:], in0=r2[:], in1=neglam[:], op=ALU.mult),
                             reads=["r2", "neglam"], writes=["lam2"])
                        S.op("dve", lambda e: e.scalar_tensor_tensor(out=o2[:], in0=ptr[:, 1, 0:64], scalar=lam2[:], in1=o1[:],
                                                                     op0=ALU.mult, op1=ALU.add),
                             reads=["ptr", "lam2", "o1"], writes=["o2"])
                        S.op("act", lambda e: e.activation(out=junk[:], in_=o2[:], func=AF.Square, accum_out=ss[:]),
                             reads=["o2"], writes=["ajunk", "ss"])
                        rstd_ops(S, ss[:], rs[:], 64, "ss", "rs")
                        S.op("dve", lambda e, obi=obi: e.tensor_scalar(out=ob[obi][:], in0=o2[:], scalar1=rs[:], scalar2=None,
                                                                       op0=ALU.mult), reads=["o2", "rs"], writes=["ob%d" % obi])
                        col0 = h * 64
                    else:
                        S.op("act", lambda e: e.activation(out=junk[:], in_=o1[:], func=AF.Square, accum_out=ss[:]),
                             reads=["o1"], writes=["ajunk", "ss"])
                        S.op("dve", lambda e, tile_i=tile_i: e.tensor_tensor(
                            out=ssacc[:, tile_i:tile_i + 1], in0=ssacc[:, tile_i:tile_i + 1], in1=ss[:], op=ALU.add),
                            reads=["ss", "ssacc"], writes=["ssacc"])
                        S.op("dve", lambda e, obi=obi: e.tensor_copy(out=ob[obi][:], in_=o1[:]),
                             reads=["o1"], writes=["ob%d" % obi])
                        col0 = 512 + h * 64
                    S.dma("pool", lambda e, obi=obi, tok0=tok0, col0=col0: e.dma_start(
                        out=D.AO[tok0:tok0 + 128, col0:col0 + 64], in_=ob[obi][:]),
                        reads=["ob%d" % obi], writes=["AO"])
                for qt in range(4):
                    deferred.append(lambda qt=qt, f=epi: f(qt))
        while deferred:
            deferred.pop(0)()
        if kind == "m":
            S.dma("pool", lambda e: e.dma_start(out=D.SSM[:, :], in_=ssacc[:]), reads=["ssacc"], writes=["SSM"])
        S.emit()


def phase_final_only(nc, D):
    with contextlib.ExitStack() as st:
        T = lambda name, shape, dt: st.enter_context(nc.sbuf_tensor(name, list(shape), dt))
        S = Sched(nc)
        g_fin = T("g_fin", [128, 1024], F32)
        xt = [T("fxt%d" % i, [128, 1024], F32) for i in range(2)]
        ot = [T("fot%d" % i, [128, 1024], F32) for i in range(2)]
        junk = T("fjunk", [128, 1024], BF16)
        ss = T("fss", [128, 1], F32)
        rs = T("frs", [128, 1], F32)
        S.dma("sp", lambda e: e.dma_start(out=g_fin[:], in_=D.norm_final_g.partition_broadcast(128)), writes=["g_fin"])
        for t in range(NT_OWN):
            b = t % 2
            tok0 = t * 128
            S.dma("sp", lambda e, b=b, tok0=tok0: e.dma_start(out=xt[b][:], in_=D.xk[tok0:tok0 + 128, :]), writes=["fxt%d" % b])
            S.op("act", lambda e, b=b: e.activation(out=junk[:], in_=xt[b][:], func=AF.Square, accum_out=ss[:]),
                 reads=["fxt%d" % b], writes=["fjunk", "fss"])
            rstd_ops(S, ss[:], rs[:], 1024, "fss", "frs")
            S.op("dve", lambda e, b=b: e.scalar_tensor_tensor(out=ot[b][:], in0=xt[b][:], scalar=rs[:], in1=g_fin[:],
                                                              op0=ALU.mult, op1=ALU.mult),
                 reads=["fxt%d" % b, "frs", "g_fin"], writes=["fot%d" % b])
            S.dma("pool", lambda e, b=b, tok0=tok0: e.dma_start(out=D.out[tok0:tok0 + 128, :], in_=ot[b][:]),
                  reads=["fot%d" % b], writes=["out"])
        S.emit()


def build(debug=False, upto=99):
    nc = bass.Bass("TRN2", target_bir_lowering=False)
    Sched.reset(nc)
    D = declare_io(nc, debug, upto)
    nc.in_names = D.in_names
    phase1(nc, D)
    if upto >= 2:
        attn_phase(nc, D, "d")
    if upto >= 3:
        attn_phase(nc, D, "m")
    if upto >= 4:
        phase4(nc, D)
    if upto >= 5:
        phase5(nc, D)
    return nc


def host_inputs(inputs):
    x = np.asarray(inputs["x"]); mem = np.asarray(inputs["mem"]); pos = np.asarray(inputs["positions"])
    ident = np.eye(128, dtype=np.float32)
    kk = np.arange(128)[:, None]; qq = np.arange(128)[None, :]
    trimask = np.where(kk <= qq, 0.0, NEG).astype(np.float32)
    sq = lambda a: np.ascontiguousarray(np.asarray(a)[0])
    row = lambda a: np.ascontiguousarray(np.asarray(a)[0].reshape(1, -1))
    shared = {
        "ident": ident, "trimask": trimask,
        "norm_mix_g": row(inputs["norm_mix_g"]), "w_in": sq(inputs["w_in"]),
        "lam_q1": row(inputs["diff_lambda_q1"]), "lam_k1": row(inputs["diff_lambda_k1"]),
        "lam_q2": row(inputs["diff_lambda_q2"]), "lam_k2": row(inputs["diff_lambda_k2"]),
        "diff_out_g": row(inputs["diff_out_g"]), "mla_q_norm_g": row(inputs["mla_q_norm_g"]),
        "w_mla_uq": sq(inputs["w_mla_uq"]), "mla_kv_norm_g": row(inputs["mla_kv_norm_g"]),
        "w_mla_ukv": sq(inputs["w_mla_ukv"]), "mla_out_g": row(inputs["mla_out_g"]),
        "w_out": sq(inputs["w_out"]), "norm_cross_g": row(inputs["norm_cross_g"]),
        "norm_mem_g": row(inputs["norm_mem_g"]), "w_mem_q": sq(inputs["w_mem_q"]),
        "w_mem_kv": sq(inputs["w_mem_kv"]), "w_mem_o": sq(inputs["w_mem_o"]),
        "norm_ffn_g": row(inputs["norm_ffn_g"]), "w_grp": sq(inputs["w_group_router"]),
        "b_grp": row(inputs["b_group_router"]), "w_exr": sq(inputs["w_expert_router"]),
        "b_exr": row(inputs["b_expert_router"]), "w_gate": sq(inputs["w_expert_gate"]),
        "w_up": sq(inputs["w_expert_up"]), "w_down": sq(inputs["w_expert_down"]),
        "norm_final_g": np.ascontiguousarray(np.asarray(inputs["norm_final_g"]).reshape(1, -1)),
    }
    maps = []
    for core in range(8):
        b, p = core // 2, core % 2
        xb = x[b].reshape(NT_ALL, 128, 1024)
        order = list(range(p, NT_ALL, 2)) + list(range(1 - p, NT_ALL, 2))
        xk = np.ascontiguousarray(xb[order].reshape(S_ALL, 1024))
        pk = pos[b].reshape(NT_ALL, 128)[order]
        m = dict(shared)
        m["xk"] = xk
        m["postok"] = np.ascontiguousarray(pk.T.astype(np.int32))
        m["mem"] = np.ascontiguousarray(mem[b])
        m["flagmask"] = np.full((128, 128), 0.0 if p == 1 else NEG, np.float32)
        maps.append(m)
    return maps


def kernel(**inputs):
    nc = build()
    maps = host_inputs(inputs)
    res = run_bass_kernel_spmd(nc, maps, core_ids=list(range(8)))
    out = np.zeros((4, S_ALL, 1024), np.float32)
    for core in range(8):
        b, p = core // 2, core % 2
        o = np.asarray(res.results[core]["out"]).reshape(NT_OWN, 128, 1024)
        out[b].reshape(NT_ALL, 128, 1024)[p::2] = o
    return out


def phase4(nc, D):
    NTL = int(os.environ.get("P4NT", str(NT_OWN)))
    with contextlib.ExitStack() as st:
        T = lambda name, shape, dt: st.enter_context(nc.sbuf_tensor("p4_" + name, list(shape), dt))
        PS = lambda name, shape, dt: st.enter_context(nc.psum_tensor("p4_" + name, list(shape), dt))
        S = Sched(nc)
        idf = T("idf", [128, 128], F32)
        idb = T("idb", [128, 128], BF16)
        onesb = T("onesb", [128, 128], BF16)
        wb_out = T("wb_out", [128, 8, 1024], BF16)
        wb_q = T("wb_q", [128, 8, 512], BF16)
        wb_kv = T("wb_kv", [128, 8, 1024], BF16)
        wb_o = T("wb_o", [128, 4, 1024], BF16)
        wr = T("wr", [128, 8, 36], F32)
        br = T("br", [128, 36], F32)
        g_cross = T("g_cross", [128, 1024], F32)
        g_mem = T("g_mem", [128, 1024], F32)
        g_ffn = T("g_ffn", [128, 1024], F32)
        gvs = [T("gv%d" % c, [128, 1], F32) for c in range(8)]
        ssm = T("ssm", [128, NT_OWN], F32)
        memt = T("memt", [128, 1024], F32)
        memn = T("memn", [128, 1024], BF16)
        memT = T("memT", [128, 8, 256], BF16)
        KmemT = T("KmemT", [128, 4, 256], BF16)
        Vmem = T("Vmem", [128, 2, 512], BF16)
        WR = T("WR", [128, NT_OWN, 32], F32)
        ao = [T("ao%d" % i, [128, 1024], BF16) for i in range(2)]
        aoT = T("aoT", [128, 8, 128], BF16)
        xt = [T("xt%d" % i, [128, 1024], F32) for i in range(2)]
        x1 = T("x1", [128, 1024], F32)
        x2 = [T("x2%d" % i, [128, 1024], F32) for i in range(2)]
        junk = T("junk", [128, 1024], BF16)
        ss = T("ss", [128, 1], F32)
        rs = T("rs", [128, 1], F32)
        rm = T("rm", [128, 1], F32)
        hb = T("hb", [128, 1024], BF16)
        hf = T("hf", [128, 1024], F32)
        hT = T("hT", [128, 8, 128], BF16)
        h3T = [T("h3T%d" % i, [128, 8, 128], BF16) for i in range(2)]
        hTf = T("hTf", [128, 8, 128], F32)
        qT = T("qT", [128, 4, 128], BF16)
        pxT = T("pxT", [128, 8, 128], BF16)
        rl = T("rl", [128, 512], F32)
        oxn = T("oxn", [128, 4, 128], BF16)
        lg = T("lg", [128, 36], F32)
        gmax = T("gmax", [128, 1], F32)
        gsum = T("gsum", [128, 1], F32)
        ggate = T("ggate", [128, 1], F32)
        gexp = T("gexp", [128, 4], F32)
        oh = T("oh", [128, 4], F32)
        pen = T("pen", [128, 4], F32)
        pen32 = T("pen32", [128, 4, 8], F32)
        elm = T("elm", [128, 32], F32)
        elm2 = T("elm2", [128, 32], F32)
        eq1 = T("eq1", [128, 32], F32)
        eq2 = T("eq2", [128, 32], F32)
        m1 = T("m1", [128, 1], F32)
        m2 = T("m2", [128, 1], F32)
        w1 = T("w1", [128, 1], F32)
        w2 = T("w2", [128, 1], F32)

        pA = PS("pA", [128, 1024], F32)
        pB = PS("pB", [128, 1024], F32)
        pT8 = PS("pT8", [128, 8, 128], BF16)
        pQ = PS("pQ", [128, 4, 128], F32)
        pL = PS("pL", [128, 4, 128], F32)
        pR = PS("pR", [128, 512], F32)

        S.dma("sp", lambda e: e.dma_start(out=idf[:], in_=D.ident[:, :]), writes=["idf"])
        S.op("dve", lambda e: e.tensor_copy(out=idb[:], in_=idf[:]), reads=["idf"], writes=["idb"])
        S.op("dve", lambda e: e.memset(onesb[:], 1.0), writes=["onesb"])
        S.op("dve", lambda e: e.memset(WR[:], 0.0), writes=["WR"])
        S.dma("sp", lambda e: e.dma_start(out=g_cross[:], in_=D.norm_cross_g.partition_broadcast(128)), writes=["g_cross"])
        S.dma("sp", lambda e: e.dma_start(out=g_mem[:], in_=D.norm_mem_g.partition_broadcast(128)), writes=["g_mem"])
        S.dma("sp", lambda e: e.dma_start(out=g_ffn[:], in_=D.norm_ffn_g.partition_broadcast(128)), writes=["g_ffn"])
        S.dma("sp", lambda e: e.dma_start(out=ssm[:], in_=D.SSM[:, :]), writes=["ssm"])
        S.dma("sp", lambda e: e.dma_start(out=wr[:, :, 0:4], in_=D.w_grp.rearrange("(c p) g -> p c g", p=128)), writes=["wr"])
        S.dma("sp", lambda e: e.dma_start(out=wr[:, :, 4:36], in_=D.w_exr.rearrange("(c p) g -> p c g", p=128)), writes=["wr"])
        S.dma("sp", lambda e: e.dma_start(out=br[:, 0:4], in_=D.b_grp.partition_broadcast(128)), writes=["br"])
        S.dma("sp", lambda e: e.dma_start(out=br[:, 4:36], in_=D.b_exr.partition_broadcast(128)), writes=["br"])
        dg = D.diff_out_g.rearrange("o d -> d o")
        for c in range(4):
            S.dma("sp", lambda e, c=c: e.dma_start(out=gvs[c][0:64, :], in_=dg), writes=["gv%d" % c])
            S.dma("sp", lambda e, c=c: e.dma_start(out=gvs[c][64:128, :], in_=dg), writes=["gv%d" % c])
            S.op("dve", lambda e, c=c: e.tensor_single_scalar(out=gvs[c][:], in_=gvs[c][:], scalar=0.8, op=ALU.mult),
                 reads=["gv%d" % c], writes=["gv%d" % c])
        mg = D.mla_out_g.rearrange("o (c p) -> c p o", p=128)
        for c in range(4):
            S.dma("sp", lambda e, c=c: e.dma_start(out=gvs[4 + c][:], in_=mg[c]), writes=["gv%d" % (4 + c)])
        w_out_v = D.w_out.rearrange("(c p) n -> p c n", p=128)
        for c in range(8):
            S.dma("pool", lambda e, c=c: e.dma_start(out=wb_out[:, c, :], in_=w_out_v[:, c, :]), writes=["wb_out%d" % c])
            S.op("dve", lambda e, c=c: e.tensor_scalar(out=wb_out[:, c, :], in0=wb_out[:, c, :], scalar1=gvs[c][:], scalar2=None,
                                                       op0=ALU.mult), reads=["wb_out%d" % c, "gv%d" % c], writes=["wb_out%d" % c])
        S.dma("pool", lambda e: e.dma_start(out=wb_q[:], in_=D.w_mem_q.rearrange("(c p) n -> p c n", p=128)), writes=["wb_q"])
        w_kv_v = D.w_mem_kv.rearrange("(c p) n -> p c n", p=128)
        for c in range(8):
            S.dma("pool", lambda e, c=c: e.dma_start(out=wb_kv[:, c, :], in_=w_kv_v[:, c, :]), writes=["wb_kv"])
        S.dma("pool", lambda e: e.dma_start(out=wb_o[:], in_=D.w_mem_o.rearrange("(c p) n -> p c n", p=128)), writes=["wb_o"])
        wb_out_keys = ["wb_out%d" % c for c in range(8)]

        def transposes8(src, src_key, dst, dst_key, eng="dve"):
            for c in range(8):
                S.op("pe", lambda e, c=c: e.transpose(out=pT8[:, c, :], in_=src[:, c * 128:(c + 1) * 128], identity=idb[:]),
                     reads=[src_key, "idb"], writes=["pT8"])
            if eng == "dve":
                S.op("dve", lambda e: e.tensor_copy(out=dst, in_=pT8[:]), reads=["pT8"], writes=[dst_key])
            else:
                S.op("act", lambda e: e.activation(out=dst, in_=pT8[:], func=AF.Copy), reads=["pT8"], writes=[dst_key])

        def norm_tile(src, src_key, g, g_key, outs):
            S.op("act", lambda e: e.activation(out=junk[:], in_=src, func=AF.Square, accum_out=ss[:]),
                 reads=[src_key], writes=["junk", "ss"])
            rstd_ops(S, ss[:], rs[:], 1024, "ss", "rs")
            for (oap, okey) in outs:
                S.op("dve", lambda e, oap=oap: e.scalar_tensor_tensor(out=oap, in0=src, scalar=rs[:], in1=g[:],
                                                                      op0=ALU.mult, op1=ALU.mult),
                     reads=[src_key, "rs", g_key], writes=[okey])

        for mc in range(2):
            S.dma("sp", lambda e, mc=mc: e.dma_start(out=memt[:], in_=D.mem[mc * 128:(mc + 1) * 128, :]), writes=["memt"])
            norm_tile(memt[:], "memt", g_mem, "g_mem", [(memn[:], "memn")])
            transposes8(memn, "memn", memT[:, :, mc * 128:(mc + 1) * 128], "memT")
        for h in range(4):
            for c in range(8):
                S.op("pe", lambda e, h=h, c=c: e.matmul(pL[:, 0:2, :].rearrange("p a b -> p (a b)"),
                                                        lhsT=wb_kv[:, c, h * 128:(h + 1) * 128], rhs=memT[:, c, :],
                                                        start=(c == 0), stop=(c == 7)),
                     reads=["wb_kv", "memT"], writes=["pL"])
            S.op("dve", lambda e, h=h: e.tensor_copy(out=KmemT[:, h, :], in_=pL[:, 0:2, :].rearrange("p a b -> p (a b)")),
                 reads=["pL"], writes=["KmemT"])
        for mc in range(2):
            for c in range(8):
                S.op("pe", lambda e, mc=mc, c=c: e.matmul(pR[:], lhsT=memT[:, c, mc * 128:(mc + 1) * 128],
                                                          rhs=wb_kv[:, c, 512:1024], start=(c == 0), stop=(c == 7)),
                     reads=["wb_kv", "memT"], writes=["pR"])
            S.op("dve", lambda e, mc=mc: e.tensor_copy(out=Vmem[:, mc, :], in_=pR[:]), reads=["pR"], writes=["Vmem"])

        for t in range(NTL):
            b = t % 2
            tok0 = t * 128
            S.dma("sp", lambda e, b=b, tok0=tok0: e.dma_start(out=ao[b][:], in_=D.AO[tok0:tok0 + 128, :]), writes=["ao%d" % b])
            S.dma("sp", lambda e, b=b, tok0=tok0: e.dma_start(out=xt[b][:], in_=D.xk[tok0:tok0 + 128, :]), writes=["xt%d" % b])
            transposes8(ao[b], "ao%d" % b, aoT[:], "aoT")
            for half in range(2):
                for c in range(4):
                    S.op("pe", lambda e, half=half, c=c: e.matmul(pA[:, half * 512:(half + 1) * 512], lhsT=aoT[:, c, :],
                                                                  rhs=wb_out[:, c, half * 512:(half + 1) * 512],
                                                                  start=(c == 0), stop=(c == 3)),
                         reads=["aoT"] + wb_out_keys, writes=["pA"])
                for c in range(4, 8):
                    S.op("pe", lambda e, half=half, c=c: e.matmul(pB[:, half * 512:(half + 1) * 512], lhsT=aoT[:, c, :],
                                                                  rhs=wb_out[:, c, half * 512:(half + 1) * 512],
                                                                  start=(c == 4), stop=(c == 7)),
                         reads=["aoT"] + wb_out_keys, writes=["pB"])
            S.op("dve", lambda e, t=t: e.tensor_copy(out=ss[:], in_=ssm[:, t:t + 1]), reads=["ssm"], writes=["ss"])
            S.op("act", lambda e: e.activation(out=rm[:], in_=ss[:], func=AF.Ln, scale=1.0 / 512, bias=EPS),
                 reads=["ss"], writes=["rm"])
            S.op("act", lambda e: e.activation(out=rm[:], in_=rm[:], func=AF.Exp, scale=-0.5), reads=["rm"], writes=["rm"])
            for half in range(2):
                sl = slice(half * 512, (half + 1) * 512)
                S.op("dve", lambda e, sl=sl, b=b: e.tensor_tensor(out=x1[:, sl], in0=pA[:, sl], in1=xt[b][:, sl], op=ALU.add),
                     reads=["pA", "xt%d" % b], writes=["x1"])
                S.op("dve", lambda e, sl=sl: e.scalar_tensor_tensor(out=x1[:, sl], in0=pB[:, sl], scalar=rm[:], in1=x1[:, sl],
                                                                    op0=ALU.mult, op1=ALU.add),
                     reads=["pB", "rm", "x1"], writes=["x1"])
            norm_tile(x1[:], "x1", g_cross, "g_cross", [(hb[:], "hb")])
            transposes8(hb, "hb", hT[:], "hT")
            for h in range(4):
                for c in range(8):
                    S.op("pe", lambda e, h=h, c=c: e.matmul(pQ[:, h, :], lhsT=wb_q[:, c, h * 128:(h + 1) * 128], rhs=hT[:, c, :],
                                                            start=(c == 0), stop=(c == 7)),
                         reads=["wb_q", "hT"], writes=["pQ"])
            S.op("act", lambda e: e.activation(out=qT[:], in_=pQ[:], func=AF.Copy, scale=SC_X), reads=["pQ"], writes=["qT"])
            pAv = pA[:].rearrange("p (a b) -> p a b", b=128)
            for h in range(4):
                for mc in range(2):
                    S.op("pe", lambda e, h=h, mc=mc: e.matmul(pAv[:, h * 2 + mc, :], lhsT=KmemT[:, h, mc * 128:(mc + 1) * 128],
                                                              rhs=qT[:, h, :], start=True, stop=True),
                         reads=["KmemT", "qT"], writes=["pA"])
            S.op("act", lambda e: e.activation(out=pxT[:].rearrange("p a b -> p (a b)"), in_=pA[:], func=AF.Exp),
                 reads=["pA"], writes=["pxT"])
            for h in range(4):
                for mc in range(2):
                    S.op("pe", lambda e, h=h, mc=mc: e.matmul(pQ[:, h, :], lhsT=Vmem[:, mc, h * 128:(h + 1) * 128],
                                                              rhs=pxT[:, h * 2 + mc, :], start=(mc == 0), stop=(mc == 1)),
                         reads=["Vmem", "pxT"], writes=["pQ"])
                for mc in range(2):
                    S.op("pe", lambda e, h=h, mc=mc: e.matmul(pL[:, h, :], lhsT=onesb[:], rhs=pxT[:, h * 2 + mc, :],
                                                              start=(mc == 0), stop=(mc == 1)),
                         reads=["onesb", "pxT"], writes=["pL"])
            S.op("dve", lambda e: e.reciprocal(out=rl[:], in_=pL[:].rearrange("p a b -> p (a b)")), reads=["pL"], writes=["rl"])
            S.op("dve", lambda e: e.tensor_tensor(out=oxn[:].rearrange("p a b -> p (a b)"),
                                                  in0=pQ[:].rearrange("p a b -> p (a b)"), in1=rl[:], op=ALU.mult),
                 reads=["pQ", "rl"], writes=["oxn"])
            for half in range(2):
                for h in range(4):
                    S.op("pe", lambda e, half=half, h=h: e.matmul(pB[:, half * 512:(half + 1) * 512], lhsT=oxn[:, h, :],
                                                                  rhs=wb_o[:, h, half * 512:(half + 1) * 512],
                                                                  start=(h == 0), stop=(h == 3)),
                         reads=["oxn", "wb_o"], writes=["pB"])
            for half in range(2):
                sl = slice(half * 512, (half + 1) * 512)
                S.op("dve", lambda e, sl=sl, b=b: e.tensor_tensor(out=x2[b][:, sl], in0=pB[:, sl], in1=x1[:, sl], op=ALU.add),
                     reads=["pB", "x1"], writes=["x2%d" % b])
            S.dma("pool", lambda e, b=b, tok0=tok0: e.dma_start(out=D.X2[tok0:tok0 + 128, :], in_=x2[b][:]),
                  reads=["x2%d" % b], writes=["X2"])
            norm_tile(x2[b][:], "x2%d" % b, g_ffn, "g_ffn", [(hf[:], "hf"), (hb[:], "hb")])
            transposes8(hb, "hb", h3T[b][:], "h3T%d" % b, eng="act")
            S.dma("pool", lambda e, b=b, tok0=tok0: e.dma_start(out=D.H3T[:, :, tok0:tok0 + 128], in_=h3T[b][:]),
                  reads=["h3T%d" % b], writes=["H3T"])
            pBv = pB[:].rearrange("p (a b) -> p a b", b=128)
            for c in range(8):
                S.op("pe", lambda e, c=c: e.transpose(out=pBv[:, c, :], in_=hf[:, c * 128:(c + 1) * 128], identity=idf[:]),
                     reads=["hf", "idf"], writes=["pB"])
            S.op("act", lambda e: e.activation(out=hTf[:].rearrange("p a b -> p (a b)"), in_=pB[:], func=AF.Copy),
                 reads=["pB"], writes=["hTf"])
            for c in range(8):
                S.op("pe", lambda e, c=c: e.matmul(pR[:, 0:36], lhsT=hTf[:, c, :], rhs=wr[:, c, :], start=(c == 0), stop=(c == 7)),
                     reads=["hTf", "wr"], writes=["pR"])
            S.op("dve", lambda e: e.tensor_tensor(out=lg[:], in0=pR[:, 0:36], in1=br[:], op=ALU.add),
                 reads=["pR", "br"], writes=["lg"])
            S.op("dve", lambda e: e.reduce_max(out=gmax[:], in_=lg[:, 0:4], axis=mybir.AxisListType.X), reads=["lg"], writes=["gmax"])
            S.op("dve", lambda e: e.tensor_scalar(out=oh[:], in0=lg[:, 0:4], scalar1=gmax[:], scalar2=None, op0=ALU.is_equal),
                 reads=["lg", "gmax"], writes=["oh"])
            S.op("dve", lambda e: e.tensor_scalar(out=gexp[:], in0=lg[:, 0:4], scalar1=gmax[:], scalar2=None, op0=ALU.subtract),
                 reads=["lg", "gmax"], writes=["gexp"])
            S.op("act", lambda e: e.activation(out=gexp[:], in_=gexp[:], func=AF.Exp, accum_out=gsum[:]),
                 reads=["gexp"], writes=["gexp", "gsum"])
            S.op("dve", lambda e: e.reciprocal(out=ggate[:], in_=gsum[:]), reads=["gsum"], writes=["ggate"])
            S.op("dve", lambda e: e.tensor_scalar(out=pen[:], in0=oh[:], scalar1=-1.0, scalar2=1e30, op0=ALU.add, op1=ALU.mult),
                 reads=["oh"], writes=["pen"])
            for j in range(8):
                S.op("dve", lambda e, j=j: e.tensor_copy(out=pen32[:, :, j], in_=pen[:]), reads=["pen"], writes=["pen32"])
            S.op("dve", lambda e: e.tensor_tensor(out=elm[:], in0=lg[:, 4:36], in1=pen32[:].rearrange("p a b -> p (a b)"), op=ALU.add),
                 reads=["lg", "pen32"], writes=["elm"])
            S.op("dve", lambda e: e.reduce_max(out=m1[:], in_=elm[:], axis=mybir.AxisListType.X), reads=["elm"], writes=["m1"])
            S.op("dve", lambda e: e.tensor_scalar(out=eq1[:], in0=elm[:], scalar1=m1[:], scalar2=None, op0=ALU.is_equal),
                 reads=["elm", "m1"], writes=["eq1"])
            S.op("dve", lambda e: e.scalar_tensor_tensor(out=elm2[:], in0=eq1[:], scalar=-1e30, in1=elm[:], op0=ALU.mult, op1=ALU.add),
                 reads=["eq1", "elm"], writes=["elm2"])
            S.op("dve", lambda e: e.reduce_max(out=m2[:], in_=elm2[:], axis=mybir.AxisListType.X), reads=["elm2"], writes=["m2"])
            S.op("dve", lambda e: e.tensor_scalar(out=eq2[:], in0=elm2[:], scalar1=m2[:], scalar2=None, op0=ALU.is_equal),
                 reads=["elm2", "m2"], writes=["eq2"])
            S.op("dve", lambda e: e.tensor_tensor(out=w1[:], in0=m2[:], in1=m1[:], op=ALU.subtract), reads=["m1", "m2"], writes=["w1"])
            S.op("act", lambda e: e.activation(out=w1[:], in_=w1[:], func=AF.Exp), reads=["w1"], writes=["w1"])
            S.op("dve", lambda e: e.tensor_single_scalar(out=w1[:], in_=w1[:], scalar=1.0, op=ALU.add), reads=["w1"], writes=["w1"])
            S.op("dve", lambda e: e.reciprocal(out=w1[:], in_=w1[:]), reads=["w1"], writes=["w1"])
            S.op("dve", lambda e: e.tensor_scalar(out=w2[:], in0=w1[:], scalar1=-1.0, scalar2=1.0, op0=ALU.mult, op1=ALU.add),
                 reads=["w1"], writes=["w2"])
            S.op("dve", lambda e: e.tensor_tensor(out=w1[:], in0=w1[:], in1=ggate[:], op=ALU.mult), reads=["w1", "ggate"], writes=["w1"])
            S.op("dve", lambda e: e.tensor_tensor(out=w2[:], in0=w2[:], in1=ggate[:], op=ALU.mult), reads=["w2", "ggate"], writes=["w2"])
            S.op("dve", lambda e: e.tensor_scalar(out=eq1[:], in0=eq1[:], scalar1=w1[:], scalar2=None, op0=ALU.mult),
                 reads=["eq1", "w1"], writes=["eq1"])
            S.op("dve", lambda e, t=t: e.scalar_tensor_tensor(out=WR[:, t, :], in0=eq2[:], scalar=w2[:], in1=eq1[:],
                                                              op0=ALU.mult, op1=ALU.add),
                 reads=["eq2", "w2", "eq1"], writes=["WR"])
        S.dma("pool", lambda e: e.dma_start(out=D.WRD[:, :, :], in_=WR[:]), reads=["WR"], writes=["WRD"])
        S.emit()


def phase5(nc, D):
    NE = int(os.environ.get("P5NE", "32"))
    NHALF = int(os.environ.get("P5NH", "2"))
    TPH = int(os.environ.get("P5TPH", "16"))
    with contextlib.ExitStack() as st:
        T = lambda name, shape, dt: st.enter_context(nc.sbuf_tensor("p5_" + name, list(shape), dt))
        PS = lambda name, shape, dt: st.enter_context(nc.psum_tensor("p5_" + name, list(shape), dt))
        S = Sched(nc)
        idb = T("idb", [128, 128], BF16)
        idf = T("idf", [128, 128], F32)
        hT = T("hT", [128, 8, S_OWN], BF16)
        WR = T("WR", [128, NT_OWN, 32], F32)
        yacc = T("yacc", [128, 16, 1024], F32)
        wgu = [T("wgu%d" % i, [128, 8, 512], BF16) for i in range(2)]
        wd = [T("wd%d" % i, [128, 2, 1024], BF16) for i in range(2)]
        sg = [T("sg%d" % i, [128, 256], F32) for i in range(2)]
        hid = [T("hid%d" % i, [128, 256], BF16) for i in range(2)]
        hidT = [T("hidT%d" % i, [128, 2, 128], BF16) for i in range(2)]
        wsc = [T("wsc%d" % i, [128, 1], F32) for i in range(2)]
        g_fin = T("g_fin", [128, 1024], F32)
        xt = [T("xt%d" % i, [128, 1024], F32) for i in range(2)]
        junk = T("junk", [128, 1024], BF16)
        ss = T("ss", [128, 1], F32)
        rs = T("rs", [128, 1], F32)
        pg = [PS("pg%d" % i, [128, 512], F32) for i in range(2)]
        pt = [PS("pt%d" % i, [128, 8, 128], BF16) for i in range(2)]
        pdn = [PS("pdn%d" % i, [128, 1024], F32) for i in range(2)]

        S.dma("sp", lambda e: e.dma_start(out=idf[:], in_=D.ident[:, :]), writes=["idf"])
        S.op("dve", lambda e: e.tensor_copy(out=idb[:], in_=idf[:]), reads=["idf"], writes=["idb"])
        S.dma("sp", lambda e: e.dma_start(out=g_fin[:], in_=D.norm_final_g.partition_broadcast(128)), writes=["g_fin"])
        S.dma("sp", lambda e: e.dma_start(out=WR[:], in_=D.WRD[:, :, :]), writes=["WR"])
        NHT = int(os.environ.get("P5HT", str(S_OWN)))
        for c in range(8):
            S.dma("sp", lambda e, c=c: e.dma_start(out=hT[:, c, 0:NHT], in_=D.H3T[:, c, 0:NHT]), writes=["hT"])
        def load_w(ex, wb):
            S.dma("pool", lambda e: e.dma_start(
                out=wgu[wb][:, :, 0:256], in_=D.w_gate[ex].rearrange("(c p) f -> p c f", p=128)), writes=["wgu%d" % wb])
            S.dma("pool", lambda e: e.dma_start(
                out=wgu[wb][:, :, 256:512], in_=D.w_up[ex].rearrange("(c p) f -> p c f", p=128)), writes=["wgu%d" % wb])
            S.dma("pool", lambda e: e.dma_start(
                out=wd[wb][:], in_=D.w_down[ex].rearrange("(c p) n -> p c n", p=128)), writes=["wd%d" % wb])

        def stage1a(u):
            ex, wb, lt, t, gb = u
            for c in range(8):
                S.op("pe", lambda e, c=c: e.matmul(
                    pg[gb][:], lhsT=hT[:, c, t * 128:(t + 1) * 128], rhs=wgu[wb][:, c, :], start=(c == 0), stop=(c == 7)),
                    reads=["hT", "wgu%d" % wb], writes=["pg%d" % gb])

        def stage1b(u):
            ex, wb, lt, t, gb = u
            S.op("act", lambda e: e.activation(out=sg[gb][:], in_=pg[gb][:, 0:256], func=AF.Silu),
                 reads=["pg%d" % gb], writes=["sg%d" % gb])
            S.op("act", lambda e: e.activation(out=wsc[gb][:], in_=WR[:, t, ex:ex + 1], func=AF.Copy),
                 reads=["WR"], writes=["wsc%d" % gb])
            S.op("dve", lambda e: e.scalar_tensor_tensor(out=hid[gb][:], in0=pg[gb][:, 256:512], scalar=wsc[gb][:],
                                                         in1=sg[gb][:], op0=ALU.mult, op1=ALU.mult),
                 reads=["pg%d" % gb, "wsc%d" % gb, "sg%d" % gb], writes=["hid%d" % gb])

        def stage2a(u):
            ex, wb, lt, t, gb = u
            for fc in range(2):
                S.op("pe", lambda e, fc=fc: e.transpose(out=pt[gb][:, fc, :], in_=hid[gb][:, fc * 128:(fc + 1) * 128],
                                                        identity=idb[:]),
                     reads=["hid%d" % gb, "idb"], writes=["pt%d" % gb])
            S.op("act", lambda e: e.activation(out=hidT[gb][:], in_=pt[gb][:, 0:2, :], func=AF.Copy),
                 reads=["pt%d" % gb], writes=["hidT%d" % gb])

        def stage2b(u):
            ex, wb, lt, t, gb = u
            for h2 in range(2):
                for fc in range(2):
                    S.op("pe", lambda e, h2=h2, fc=fc: e.matmul(
                        pdn[gb][:, h2 * 512:(h2 + 1) * 512], lhsT=hidT[gb][:, fc, :],
                        rhs=wd[wb][:, fc, h2 * 512:(h2 + 1) * 512], start=(fc == 0), stop=(fc == 1)),
                        reads=["hidT%d" % gb, "wd%d" % wb], writes=["pdn%d" % gb])
            for h2 in range(2):
                sl = slice(h2 * 512, (h2 + 1) * 512)
                if ex == 0:
                    S.op("dve", lambda e, sl=sl: e.tensor_copy(out=yacc[:, lt, sl], in_=pdn[gb][:, sl]),
                         reads=["pdn%d" % gb], writes=["yacc%d" % lt])
                else:
                    S.op("dve", lambda e, sl=sl: e.tensor_tensor(out=yacc[:, lt, sl], in0=pdn[gb][:, sl],
                                                                 in1=yacc[:, lt, sl], op=ALU.add),
                         reads=["pdn%d" % gb, "yacc%d" % lt], writes=["yacc%d" % lt])

        cnt = 0
        wcount = 0
        for half in range(NHALF):
            load_w(0, wcount % 2)
            p1 = None
            p2 = None
            for ex in range(NE):
                wb = wcount % 2
                wcount += 1
                for lt in range(TPH):
                    t = half * 16 + lt
                    u = (ex, wb, lt, t, cnt % 2)
                    cnt += 1
                    stage1a(u)
                    if p1 is not None:
                        stage2a(p1)
                    stage1b(u)
                    if p2 is not None:
                        stage2b(p2)
                    p2 = p1
                    p1 = u
                    if lt == 1 and ex + 1 < NE:
                        load_w(ex + 1, wcount % 2)
            stage2a(p1)
            if p2 is not None:
                stage2b(p2)
            stage2b(p1)
            for lt in range(TPH):
                t = half * 16 + lt
                tok0 = t * 128
                b = lt % 2
                S.dma("sp", lambda e, b=b, tok0=tok0: e.dma_start(out=xt[b][:], in_=D.X2[tok0:tok0 + 128, :]), writes=["xt%d" % b])
                S.op("pool", lambda e, b=b, lt=lt: e.tensor_tensor(out=xt[b][:], in0=xt[b][:], in1=yacc[:, lt, :], op=ALU.add),
                     reads=["xt%d" % b, "yacc%d" % lt], writes=["xt%d" % b])
                S.op("act", lambda e, b=b: e.activation(out=junk[:], in_=xt[b][:], func=AF.Square, accum_out=ss[:]),
                     reads=["xt%d" % b], writes=["junk", "ss"])
                rstd_ops(S, ss[:], rs[:], 1024, "ss", "rs")
                S.op("dve", lambda e, b=b: e.scalar_tensor_tensor(out=xt[b][:], in0=xt[b][:], scalar=rs[:], in1=g_fin[:],
                                                                  op0=ALU.mult, op1=ALU.mult),
                     reads=["xt%d" % b, "rs", "g_fin"], writes=["xt%d" % b])
                S.dma("sp", lambda e, b=b, tok0=tok0: e.dma_start(out=D.out[tok0:tok0 + 128, :], in_=xt[b][:]),
                      reads=["xt%d" % b], writes=["out"])
        S.emit()
```
